# Optimizing a Trainium2 kernel written in Bass

```python
import jax, jax.numpy as jnp
from jax import lax
import numpy as np

D_MODEL = 1024
BATCH = 2
SEQ = 8192
DEPTH = 2
DEC_BATCH = 128
DEC_SEQ = 4
PAST_LEN = 2048
PAGE_SIZE = 128

HEAD_DIM = 64
ATT_WIDTH = D_MODEL // 2
N_ATT_HEADS = ATT_WIDTH // HEAD_DIM
CONV_CH = D_MODEL - ATT_WIDTH
CONV_WIDTH = 31
DILATION_PATTERNS = ((128, 1), (512, 4), (2048, 16))
MAX_WINDOW = max(w for w, _ in DILATION_PATTERNS)
ROT_DIM = HEAD_DIM // 4
ROPE_THETA = 500000.0
ATTN_SCALE = HEAD_DIM ** -0.5
PROJ_WIDTH = 3 * ATT_WIDTH + 2 * CONV_CH
D_FF = 7 * D_MODEL // 2
N_EXPERTS = 8
TOP_K = 2
D_FF_EXPERT = 7 * D_MODEL // 2
N_DENSE = (DEPTH + 1) // 2
N_MOE = DEPTH // 2
Q_BLOCK = 128
EPS = 1e-6

kernel_name = "hymba_dilated_conformer_decoder_step"


def rmsnorm(x, g):
    x32 = x.astype(jnp.float32)
    y = x32 * lax.rsqrt(jnp.mean(x32 * x32, axis=-1, keepdims=True) + EPS) * g.astype(jnp.float32)
    return y.astype(x.dtype)


def layernorm(x, g, b):
    x32 = x.astype(jnp.float32)
    mu = jnp.mean(x32, axis=-1, keepdims=True)
    xc = x32 - mu
    var = jnp.mean(xc * xc, axis=-1, keepdims=True)
    y = xc * lax.rsqrt(var + EPS) * g.astype(jnp.float32) + b.astype(jnp.float32)
    return y.astype(x.dtype)


def rope_partial(x, pos):
    half = ROT_DIM // 2
    inv_freq = ROPE_THETA ** (-jnp.arange(half, dtype=jnp.float32) * 2.0 / ROT_DIM)
    ang = pos.astype(jnp.float32)[:, None] * inv_freq[None, :]
    cos = jnp.cos(ang)[None, :, None, :]
    sin = jnp.sin(ang)[None, :, None, :]
    xr = x[..., :ROT_DIM].astype(jnp.float32)
    x1, x2 = xr[..., :half], xr[..., half:]
    rot = jnp.concatenate([x1 * cos - x2 * sin, x2 * cos + x1 * sin], axis=-1).astype(x.dtype)
    return jnp.concatenate([rot, x[..., ROT_DIM:]], axis=-1)


def dilated_attention(q, keys, vals, qrow):
    B, T, H, Dh = q.shape
    blk = Q_BLOCK if T % Q_BLOCK == 0 else T
    nb = T // blk
    qb = q.reshape(B, nb, blk, H, Dh).transpose(1, 0, 2, 3, 4)
    rb = qrow.reshape(nb, blk)

    def one_block(args):
        q_blk, r_blk = args
        outs, lses = [], []
        for window, dil in DILATION_PATTERNS:
            offs = dil * jnp.arange(window // dil + 1, dtype=jnp.int32)
            idx = r_blk[:, None] - offs[None, :]
            valid = idx >= 0
            idx = jnp.maximum(idx, 0)
            kg = keys[:, idx]
            vg = vals[:, idx]
            s = jnp.einsum('bqhd,bqkhd->bqkh', q_blk, kg).astype(jnp.float32) * ATTN_SCALE
            s = jnp.where(valid[None, :, :, None], s, -jnp.inf)
            lse = jax.nn.logsumexp(s, axis=2)
            prob = jnp.exp(s - lse[:, :, None, :])
            outs.append(jnp.einsum('bqkh,bqkhd->bqhd', prob, vg.astype(jnp.float32)))
            lses.append(lse)
        wts = jax.nn.softmax(jnp.stack(lses), axis=0)
        return jnp.sum(wts[..., None] * jnp.stack(outs), axis=0).astype(q.dtype)

    out = lax.map(one_block, (qb, rb))
    return out.transpose(1, 0, 2, 3, 4).reshape(B, T, H, Dh)


def causal_dwconv(u_full, w, b):
    y = lax.conv_general_dilated(u_full, w[:, None, :], window_strides=(1,), padding='VALID',
                                 dimension_numbers=('NWC', 'WIO', 'NWC'),
                                 feature_group_count=u_full.shape[-1])
    return y + b


def swiglu(x, w_gate, w_up, w_down):
    return (jax.nn.silu(x @ w_gate) * (x @ w_up)) @ w_down


def moe_swiglu(h, w_router, w_gate, w_up, w_down):
    B, T, D = h.shape
    xf = h.reshape(B * T, D)
    logits = (xf @ w_router).astype(jnp.float32)
    top_v, top_i = lax.top_k(logits, TOP_K)
    top_w = jax.nn.softmax(top_v, axis=-1)
    gates = jnp.sum(jax.nn.one_hot(top_i, N_EXPERTS, dtype=jnp.float32) * top_w[..., None], axis=1)
    out = jnp.zeros((B * T, D), jnp.float32)
    for e in range(N_EXPERTS):
        out = out + gates[:, e:e + 1] * swiglu(xf, w_gate[e], w_up[e], w_down[e]).astype(jnp.float32)
    return out.astype(h.dtype).reshape(B, T, D)


def layer_forward(x, c, pos0, k_hist, v_hist, conv_hist, l, p):
    B, T, _ = x.shape
    mod = (jax.nn.silu(c) @ p['w_mod'][l] + p['b_mod'][l])[:, None, :]
    sh1, sc1, gt1, sh2, sc2, gt2 = jnp.split(mod, 6, axis=-1)

    h = rmsnorm(x, p['g_pre_mix'][l]) * (1 + sc1) + sh1
    proj = h @ p['w_in'][l]
    q, k, v, a, g = jnp.split(proj, [ATT_WIDTH, 2 * ATT_WIDTH, 3 * ATT_WIDTH,
                                     3 * ATT_WIDTH + CONV_CH], axis=-1)
    pos = pos0 + jnp.arange(T, dtype=jnp.int32)
    q = rope_partial(q.reshape(B, T, N_ATT_HEADS, HEAD_DIM), pos)
    k = rope_partial(k.reshape(B, T, N_ATT_HEADS, HEAD_DIM), pos)
    v = v.reshape(B, T, N_ATT_HEADS, HEAD_DIM)
    keys = jnp.concatenate([k_hist, k], axis=1)
    vals = jnp.concatenate([v_hist, v], axis=1)
    qrow = k_hist.shape[1] + jnp.arange(T, dtype=jnp.int32)
    att = dilated_attention(q, keys, vals, qrow).reshape(B, T, ATT_WIDTH)

    u = a * jax.nn.sigmoid(g)
    u_full = jnp.concatenate([conv_hist, u], axis=1)
    cv = causal_dwconv(u_full, p['conv_w'][l], p['conv_b'][l])
    cv = jax.nn.silu(layernorm(cv, p['conv_ln_g'][l], p['conv_ln_b'][l]))

    mix = jnp.concatenate([att, cv], axis=-1) @ p['w_out'][l]
    x = x + gt1 * rmsnorm(mix, p['g_post_mix'][l])

    h2 = rmsnorm(x, p['g_pre_ffn'][l]) * (1 + sc2) + sh2
    i = l // 2
    if l % 2 == 0:
        f = swiglu(h2, p['ffn_w_gate'][i], p['ffn_w_up'][i], p['ffn_w_down'][i])
    else:
        f = moe_swiglu(h2, p['moe_w_router'][i], p['moe_w_gate'][i], p['moe_w_up'][i], p['moe_w_down'][i])
    x = x + gt2 * rmsnorm(f, p['g_post_ffn'][l])

    keep = min(MAX_WINDOW, T)
    return x, k[:, T - keep:], v[:, T - keep:], u_full[:, -(CONV_WIDTH - 1):]


def setup_inputs(seed: int = 0) -> dict:
    key = jax.random.key(seed)
    ks = jax.random.split(key, 32)
    f32 = jnp.float32
    W_BUF = min(MAX_WINDOW, PAST_LEN)

    def nrm(k, shape, s):
        return jax.random.normal(k, shape, f32) * s

    return {
        "x_prompt": nrm(ks[0], (BATCH, SEQ, D_MODEL), 1.0),
        "x_sample": nrm(ks[1], (DEC_BATCH, DEC_SEQ, D_MODEL), 1.0),
        "cache_k": nrm(ks[2], (DEPTH, DEC_BATCH, W_BUF, N_ATT_HEADS, HEAD_DIM), 1.0),
        "cache_v": nrm(ks[3], (DEPTH, DEC_BATCH, W_BUF, N_ATT_HEADS, HEAD_DIM), 1.0),
        "state_conv": nrm(ks[4], (DEPTH, DEC_BATCH, CONV_WIDTH - 1, CONV_CH), 0.5),
        "c_prompt": nrm(ks[5], (BATCH, D_MODEL), 1.0),
        "c_sample": nrm(ks[6], (DEC_BATCH, D_MODEL), 1.0),
        "w_mod": nrm(ks[7], (DEPTH, D_MODEL, 6 * D_MODEL), 0.3 * D_MODEL ** -0.5),
        "b_mod": nrm(ks[8], (DEPTH, 6 * D_MODEL), 0.02),
        "g_pre_mix": 1.0 + nrm(ks[9], (DEPTH, D_MODEL), 0.05),
        "g_post_mix": 1.0 + nrm(ks[10], (DEPTH, D_MODEL), 0.05),
        "g_pre_ffn": 1.0 + nrm(ks[11], (DEPTH, D_MODEL), 0.05),
        "g_post_ffn": 1.0 + nrm(ks[12], (DEPTH, D_MODEL), 0.05),
        "w_in": nrm(ks[13], (DEPTH, D_MODEL, PROJ_WIDTH), D_MODEL ** -0.5),
        "conv_w": nrm(ks[14], (DEPTH, CONV_WIDTH, CONV_CH), CONV_WIDTH ** -0.5),
        "conv_b": nrm(ks[15], (DEPTH, CONV_CH), 0.02),
        "conv_ln_g": 1.0 + nrm(ks[16], (DEPTH, CONV_CH), 0.05),
        "conv_ln_b": nrm(ks[17], (DEPTH, CONV_CH), 0.02),
        "w_out": nrm(ks[18], (DEPTH, D_MODEL, D_MODEL), D_MODEL ** -0.5),
        "ffn_w_gate": nrm(ks[19], (N_DENSE, D_MODEL, D_FF), D_MODEL ** -0.5),
        "ffn_w_up": nrm(ks[20], (N_DENSE, D_MODEL, D_FF), D_MODEL ** -0.5),
        "ffn_w_down": nrm(ks[21], (N_DENSE, D_FF, D_MODEL), D_FF ** -0.5),
        "moe_w_router": nrm(ks[22], (N_MOE, D_MODEL, N_EXPERTS), D_MODEL ** -0.5),
        "moe_w_gate": nrm(ks[23], (N_MOE, N_EXPERTS, D_MODEL, D_FF_EXPERT), D_MODEL ** -0.5),
        "moe_w_up": nrm(ks[24], (N_MOE, N_EXPERTS, D_MODEL, D_FF_EXPERT), D_MODEL ** -0.5),
        "moe_w_down": nrm(ks[25], (N_MOE, N_EXPERTS, D_FF_EXPERT, D_MODEL), D_FF_EXPERT ** -0.5),
    }


def reference(x_prompt, x_sample, cache_k, cache_v, state_conv, c_prompt, c_sample,
              w_mod, b_mod, g_pre_mix, g_post_mix, g_pre_ffn, g_post_ffn, w_in,
              conv_w, conv_b, conv_ln_g, conv_ln_b, w_out,
              ffn_w_gate, ffn_w_up, ffn_w_down,
              moe_w_router, moe_w_gate, moe_w_up, moe_w_down):
    p = dict(w_mod=w_mod, b_mod=b_mod, g_pre_mix=g_pre_mix, g_post_mix=g_post_mix,
             g_pre_ffn=g_pre_ffn, g_post_ffn=g_post_ffn, w_in=w_in, conv_w=conv_w, conv_b=conv_b,
             conv_ln_g=conv_ln_g, conv_ln_b=conv_ln_b, w_out=w_out,
             ffn_w_gate=ffn_w_gate, ffn_w_up=ffn_w_up, ffn_w_down=ffn_w_down,
             moe_w_router=moe_w_router, moe_w_gate=moe_w_gate, moe_w_up=moe_w_up,
             moe_w_down=moe_w_down)
    bp = x_prompt.shape[0]
    dt = x_prompt.dtype
    empty_kv = jnp.zeros((bp, 0, N_ATT_HEADS, HEAD_DIM), dt)
    zero_conv = jnp.zeros((bp, CONV_WIDTH - 1, CONV_CH), dt)

    yp, ys = x_prompt, x_sample
    kp_l, vp_l, cp_l, ks_l, vs_l, cs_l = [], [], [], [], [], []
    for l in range(DEPTH):
        yp, kp, vp, cp = layer_forward(yp, c_prompt, 0, empty_kv, empty_kv, zero_conv, l, p)
        ys, k_s, v_s, c_s = layer_forward(ys, c_sample, PAST_LEN, cache_k[l], cache_v[l],
                                          state_conv[l], l, p)
        kp_l.append(kp); vp_l.append(vp); cp_l.append(cp)
        ks_l.append(k_s); vs_l.append(v_s); cs_l.append(c_s)

    return (yp, ys, jnp.stack(kp_l), jnp.stack(vp_l), jnp.stack(cp_l),
            jnp.stack(ks_l), jnp.stack(vs_l), jnp.stack(cs_l))
```

```python
import numpy as np
from contextlib import ExitStack
import ml_dtypes
import concourse.bass as bass
import concourse.mybir as mybir
from concourse.bass_utils import run_bass_kernel_spmd

F32 = mybir.dt.float32
BF16 = mybir.dt.bfloat16
AF = mybir.ActivationFunctionType
ALU = mybir.AluOpType
AX = mybir.AxisListType

COMPUTE = ("pe", "act", "dve", "pool")
NSLOT = 2


class Sched:
    def __init__(self, nc, stack):
        self.nc = nc
        self.eng = {"pe": nc.tensor, "act": nc.scalar, "dve": nc.vector, "pool": nc.gpsimd, "sp": nc.sync}
        self.ops = {e: [] for e in self.eng}
        self.tl = {e: stack.enter_context(nc.semaphore("tl_" + e)) for e in COMPUTE}
        self.cnt = {e: 0 for e in COMPUTE}
        self.slots = {q: [stack.enter_context(nc.semaphore(f"dq_{q}_{i}")) for i in range(NSLOT)]
                      for q in ("sp", "pool", "act")}
        self.slot_uses = {q: [0] * NSLOT for q in self.slots}
        self.slot_next = {q: 0 for q in self.slots}
        self.last_w = {}
        self.reads = {}
        self.seen = {e: {} for e in self.eng}

    def _need(self, e, ev, waits, force=False):
        if ev is None:
            return
        kind, ident, val = ev
        if (not force) and kind == "tl" and ident == e and e == "pe":
            return
        key = (kind, ident)
        if self.seen[e].get(key, 0) >= val:
            return
        self.seen[e][key] = val
        waits[key] = max(waits.get(key, 0), val)

    def _deps(self, e, reads, writes):
        waits = {}
        for k in reads:
            for ev in self.last_w.get(k, ()):
                self._need(e, ev, waits)
        for k in writes:
            for ev in self.last_w.get(k, ()):
                self._need(e, ev, waits)
            for ev in self.reads.get(k, ()):
                self._need(e, ev, waits)
        return waits

    def _commit(self, evs, reads, writes):
        for k in reads:
            self.reads.setdefault(k, []).extend(evs)
        for k in writes:
            self.last_w[k] = list(evs)
            self.reads[k] = []

    def op(self, e, fn, reads=(), writes=()):
        waits = self._deps(e, reads, writes)
        self.cnt[e] += 1
        ev = ("tl", e, self.cnt[e])
        self.ops[e].append((waits, fn, ("tl", e)))
        self._commit([ev], reads, writes)
        return ev

    def dma(self, q, out, in_, reads=(), writes=(), **kw):
        waits = self._deps(q, reads, writes)
        osh = tuple(out.shape)
        parts = []
        if len(osh) == 3 and tuple(in_.shape) == osh and osh[0] * osh[1] > 256 and osh[1] > 1:
            step = max(1, 256 // osh[0])
            for a0 in range(0, osh[1], step):
                a1 = min(osh[1], a0 + step)
                parts.append((out[:, a0:a1, :], in_[:, a0:a1, :]))
        else:
            parts.append((out, in_))
        evs = []
        for i, (o_, i_) in enumerate(parts):
            evs.append(self._dma1(q, o_, i_, waits if i == 0 else {}, **kw))
        self._commit(evs, reads, writes)
        return evs[-1]

    def _dma1(self, q, out, in_, waits, **kw):
        s = self.slot_next[q]
        self.slot_next[q] = (s + 1) % NSLOT
        waits = dict(waits)
        if self.slot_uses[q][s] > 0:
            self._need(q, ("dma", (q, s), 16 * self.slot_uses[q][s]), waits)
        self.slot_uses[q][s] += 1
        ev = ("dma", (q, s), 16 * self.slot_uses[q][s])
        fn = lambda eng, out=out, in_=in_, kw=kw: eng.dma_start(out=out, in_=in_, **kw)
        self.ops[q].append((waits, fn, ("dma", (q, s))))
        return ev

    def _all_events(self):
        evs = [("tl", e, self.cnt[e]) for e in COMPUTE if self.cnt[e] > 0]
        for q in self.slots:
            for s in range(NSLOT):
                if self.slot_uses[q][s] > 0:
                    evs.append(("dma", (q, s), 16 * self.slot_uses[q][s]))
        return evs

    def barrier(self):
        evs = self._all_events()
        for e in self.eng:
            waits = {}
            for ev in evs:
                self._need(e, ev, waits, force=True)
            if waits:
                self.ops[e].append((waits, None, None))
        self.last_w = {}
        self.reads = {}

    def final_wait(self, e="sp"):
        waits = {}
        for ev in self._all_events():
            self._need(e, ev, waits, force=True)
        self.ops[e].append((waits, None, None))

    def _sem(self, key):
        kind, ident = key
        if kind == "tl":
            return self.tl[ident]
        q, s = ident
        return self.slots[q][s]

    def flush(self):
        nc = self.nc
        ops = self.ops
        self.ops = {e: [] for e in self.eng}
        with nc.Block() as block:
            def mk(ename):
                def body(eng):
                    for waits, fn, sig in ops[ename]:
                        for key, val in waits.items():
                            eng.wait_ge(self._sem(key), val)
                        if fn is None:
                            continue
                        inst = fn(eng)
                        if sig[0] == "tl":
                            inst.then_inc(self.tl[sig[1]], 1)
                        else:
                            inst.then_inc(self._sem(sig), 16)
                return body
            block.tensor(mk("pe"))
            block.scalar(mk("act"))
            block.vector(mk("dve"))
            block.gpsimd(mk("pool"))
            block.sync(mk("sp"))


D = 1024
NPT = 48
NT = 49
TALL = NT * 128
DFF = 3584
NEG = -30000.0
EPS = 1e-6


def build_nc():
    nc = bass.Bass("TRN2", target_bir_lowering=False)

    def din(name, shape, dt=F32):
        return nc.dram_tensor(name, list(shape), dt, kind="ExternalInput")

    def dout(name, shape, dt=F32):
        return nc.dram_tensor(name, list(shape), dt, kind="ExternalOutput")

    def dscr(name, shape, dt):
        return nc.dram_tensor(name, list(shape), dt, kind="Internal")

    xin = din("xin", [TALL, D])
    cexp = din("cexp", [2, 128, D])
    ck = din("ck", [2, 16, 2048, 512])
    cv = din("cv", [2, 16, 2048, 512])
    sconv = din("sconv", [2, 16, 30, 512])
    flags = din("flags", [128, 4])
    rope = din("rope", [TALL, 4, 128])
    maskin = din("maskin", [128, 1056])
    w_mod = din("w_mod", [2, D, 6 * D]); b_mod = din("b_mod", [2, 6 * D])
    g_pre_mix = din("g_pre_mix", [2, D]); g_post_mix = din("g_post_mix", [2, D])
    g_pre_ffn = din("g_pre_ffn", [2, D]); g_post_ffn = din("g_post_ffn", [2, D])
    w_in = din("w_in", [2, D, 2560])
    conv_wT = din("conv_wT", [2, 512, 31]); conv_b = din("conv_b", [2, 512])
    conv_ln_g = din("conv_ln_g", [2, 512]); conv_ln_b = din("conv_ln_b", [2, 512])
    w_out = din("w_out", [2, D, D])
    ffn_w_gate = din("ffn_w_gate", [1, D, DFF]); ffn_w_up = din("ffn_w_up", [1, D, DFF])
    ffn_w_down = din("ffn_w_down", [1, DFF, D])
    moe_w_router = din("moe_w_router", [1, D, 8])
    moe_w_gate = din("moe_w_gate", [1, 8, D, DFF]); moe_w_up = din("moe_w_up", [1, 8, D, DFF])
    moe_w_down = din("moe_w_down", [1, 8, DFF, D])
    o_y = dout("o_y", [2048 + 128, D])
    o_k = dout("o_k", [2, 2048 + 128, 512]); o_v = dout("o_v", [2, 2048 + 128, 512])
    o_cp = dout("o_cp", [2, 32, 512]); o_cs = dout("o_cs", [2, 16, 30, 512])
    XA = dscr("XA", [TALL, D], F32); XB = dscr("XB", [TALL, D], F32)
    QT = dscr("QT", [512, TALL], BF16); KT = dscr("KT", [512, TALL], BF16)
    VV = dscr("VV", [TALL, 512], BF16)
    UT = dscr("UT", [512, TALL], BF16)
    CAT = dscr("CAT", [1024, TALL], BF16)
    H2T = dscr("H2T", [1024, TALL], BF16)
    GATES = dscr("GATES", [TALL, 8], F32)
    MODD = dscr("MODD", [2, 6, 128, D], F32)

    st = ExitStack()
    with st:
        S = Sched(nc, st)

        cur = [st]

        usage = [0]
        umax = [0, ""]

        def sb(name, shape, dt):
            n = 1
            for d_ in shape[1:]:
                n *= d_
            n *= 2 if dt == BF16 else 4
            n = (n + 31) // 32 * 32
            usage[-1] += n
            tot = sum(usage)
            if tot > umax[0]:
                umax[0] = tot; umax[1] = name
            _DBG.setdefault("usage", {})[name] = tot
            return cur[-1].enter_context(nc.sbuf_tensor(name, list(shape), dt))

        def psum(name, shape, dt):
            return cur[-1].enter_context(nc.psum_tensor(name, list(shape), dt))

        import os
        nph = [0]
        maxph = int(os.environ.get("KDBG_NPHASE", "999"))

        def phase(fn):
            def wrapped(*a, **k):
                nph[0] += 1
                if nph[0] > maxph:
                    return
                S.barrier()
                with ExitStack() as pst:
                    cur.append(pst)
                    usage.append(0)
                    fn(*a, **k)
                    S.barrier()
                    S.flush()
                    cur.pop()
                    _DBG.setdefault("phase_usage", []).append((fn.__name__, sum(usage)))
                    usage.pop()
            return wrapped

        class Ring:
            def __init__(self, name, n, shape, dt, ps=False):
                self.t = [(psum if ps else sb)(f"{name}{i}", shape, dt) for i in range(n)]
                self.k = [f"{name}{i}" for i in range(n)]
                self.i = 0

            def next(self):
                j = self.i % len(self.t)
                self.i += 1
                return self.t[j], self.k[j]

        castr = Ring("caststg", 3, [128, 1024], F32)

        def load_cast(dst, src, wkey):
            sh = tuple(dst.shape)
            if len(sh) == 2:
                stg, stgk = castr.next()
                S.dma("sp", stg[:, 0:sh[1]], src, writes=[stgk])
                S.op("pool", lambda e, stg=stg: e.tensor_copy(dst, stg[:, 0:sh[1]]), reads=[stgk], writes=[wkey])
                return
            A, B = sh[1], sh[2]
            step = max(1, 1024 // B)
            for a0 in range(0, A, step):
                a1 = min(A, a0 + step)
                stg, stgk = castr.next()
                sv = stg[:, 0:(a1 - a0) * B].rearrange("p (a b) -> p a b", b=B)
                S.dma("sp", sv, src[:, a0:a1, :], writes=[stgk])
                S.op("pool", lambda e, sv=sv, a0=a0, a1=a1: e.tensor_copy(dst[:, a0:a1, :], sv), reads=[stgk], writes=[wkey])

        def bcast_row(th, off, n):
            return bass.AP(th, off, [[0, 128], [1, n]])

        ident = sb("ident", [128, 128], BF16)
        identf = sb("identf", [128, 128], F32)
        onesf = sb("onesf", [128, 128], F32)
        ones1 = sb("ones1", [128, 128], F32)
        onesb = sb("onesb", [128, 128], BF16)
        epsb = sb("epsb", [128, 1], F32)
        flg = sb("flg", [128, 4], F32)
        maskf = sb("maskf", [128, 1056], F32)
        maskb = sb("maskb", [128, 256], BF16)
        maskhB = sb("maskhB", [128, 256], BF16)
        maskhC = sb("maskhC", [128, 256], BF16)
        smask = sb("smask", [128, 800], F32)

        S.op("pool", lambda e: e.memset(identf[:], 1.0), writes=["identf"])
        S.op("pool", lambda e: e.affine_select(identf[:], identf[:], pattern=[[-1, 128]], compare_op=ALU.is_equal,
                                               fill=0.0, base=0, channel_multiplier=1), reads=["identf"], writes=["identf"])
        S.op("dve", lambda e: e.tensor_copy(ident[:], identf[:]), reads=["identf"], writes=["ident"])
        S.op("pool", lambda e: e.memset(onesf[:], 1.0 / 512), writes=["onesf"])
        S.op("pool", lambda e: e.memset(ones1[:], 1.0), writes=["ones1"])
        S.op("pool", lambda e: e.memset(onesb[:], 1.0), writes=["onesb"])
        S.op("pool", lambda e: e.memset(epsb[:], EPS), writes=["epsb"])
        S.dma("sp", flg[:], flags.ap(), writes=["flg"])
        S.dma("sp", maskf[:], maskin.ap(), writes=["maskf"])
        S.op("dve", lambda e: e.tensor_copy(maskb[:], maskf[:, 0:256]), reads=["maskf"], writes=["maskb"])
        S.op("dve", lambda e: e.tensor_scalar(maskhB[:], maskf[:, 0:256], flg[:, 0:1], None, ALU.add),
             reads=["maskf", "flg"], writes=["maskhB"])
        S.op("dve", lambda e: e.tensor_scalar(maskhC[:], maskf[:, 0:256], flg[:, 1:2], None, ALU.add),
             reads=["maskf", "flg"], writes=["maskhC"])
        S.op("dve", lambda e: e.tensor_copy(smask[:], maskf[:, 256:1056]), reads=["maskf"], writes=["smask"])

        pb = [psum(f"pb{i}", [128, 512], F32) for i in range(7)]
        pT = psum("pT", [128, 1024], BF16)
        PB = [f"pb{i}" for i in range(7)]

        MOD = [None, None]
        MODK = ["MODP", "MODS"]
        modcur = {}
        zt = sb("zt", [128, 8, 64], BF16)
        S.op("pool", lambda e: e.memset(zt[:], 0.0), writes=["zt"])
        S.dma("sp", CAT.ap()[:, NPT * 128 + 64:NPT * 128 + 128].rearrange("(c p) t -> p c t", p=128), zt[:], reads=["zt"])

        xr = Ring("xr", 2, [128, D], F32)
        tmpr = Ring("tmpr", 2, [128, D], F32)
        hbr = Ring("hbr", 2, [128, D], BF16)
        str_ = Ring("st", 4, [128, 8], F32)
        junk = sb("junk", [128, D], BF16)

        def rms_stats(src_aps, skeys, nfeat):
            stt, sk = str_.next()
            for i, (a, k) in enumerate(zip(src_aps, skeys)):
                S.op("act", lambda e, a=a, i=i: e.activation(junk[:, 0:a.shape[-1]], a, AF.Square, accum_out=stt[:, i:i + 1]),
                     reads=[k], writes=["junk", sk])
            if len(src_aps) == 2:
                S.op("dve", lambda e: e.tensor_tensor(stt[:, 0:1], stt[:, 0:1], stt[:, 1:2], ALU.add), reads=[sk], writes=[sk])
            S.op("act", lambda e: e.activation(stt[:, 2:3], stt[:, 0:1], AF.Sqrt, bias=epsb[:, 0:1], scale=1.0 / nfeat),
                 reads=[sk, "epsb"], writes=[sk])
            S.op("dve", lambda e: e.reciprocal(stt[:, 3:4], stt[:, 2:3]), reads=[sk], writes=[sk])
            return stt, sk

        def transpose8(src, skey, dst_ap, dkey, eng="act"):
            for kc in range(8):
                S.op("pe", lambda e, kc=kc: e.transpose(pT[:, kc * 128:(kc + 1) * 128], src[:, kc * 128:(kc + 1) * 128], ident[:]),
                     reads=[skey, "ident"], writes=["pT"])
            pv = pT[:].rearrange("p (c t) -> p c t", c=8)
            if eng == "act":
                S.op("act", lambda e: e.copy(dst_ap, pv), reads=["pT"], writes=[dkey])
            else:
                S.op("dve", lambda e: e.tensor_copy(dst_ap, pv), reads=["pT"], writes=[dkey])

        @phase
        def phase_mod(l):
            MOD[0] = sb("MODP%d" % l, [128, 6 * D], F32)
            MOD[1] = sb("MODS%d" % l, [128, 6 * D], F32)
            wmr = Ring("wm%d_" % l, 3, [128, 8, 512], BF16)
            bmr = Ring("bm%d_" % l, 2, [128, 512], F32)
            gt_ = sb("gtmp%d" % l, [128, D], F32)
            cx = sb("cx%d" % l, [128, D], F32)
            cxb = sb("cxb%d" % l, [128, D], BF16)
            cT = [sb("cT%d_%d" % (l, s), [128, 8, 128], BF16) for s in range(2)]
            for s in range(2):
                S.dma("sp", cx[:], cexp.ap()[s], writes=["cx"])
                S.op("act", lambda e: e.activation(cxb[:], cx[:], AF.Silu), reads=["cx"], writes=["cxb"])
                transpose8(cxb, "cxb", cT[s][:], "cT%d" % s)
            for nb in range(12):
                wt, wk = wmr.next()
                load_cast(wt[:], w_mod.ap()[l][:, nb * 512:(nb + 1) * 512].rearrange("(kc p) n -> p kc n", p=128), wk)
                bt, bk = bmr.next()
                S.dma("sp", bt[:], bcast_row(b_mod, l * 6 * D + nb * 512, 512), writes=[bk])
                for s in range(2):
                    pp = pb[s]
                    for kc in range(8):
                        S.op("pe", lambda e, kc=kc, s=s, pp=pp, wt=wt: e.matmul(pp[:], cT[s][:, kc, :], wt[:, kc, :], start=(kc == 0), stop=(kc == 7)),
                             reads=["cT%d" % s, wk], writes=[PB[s]])
                    S.op("dve", lambda e, s=s, pp=pp, bt=bt, nb=nb: e.tensor_tensor(MOD[s][:, nb * 512:(nb + 1) * 512], pp[:], bt[:], ALU.add),
                         reads=[PB[s], bk], writes=[MODK[s]])
            for (slot, gvec, kind) in ((1, g_pre_mix, "A"), (2, g_post_mix, "G"), (4, g_pre_ffn, "A"), (5, g_post_ffn, "G")):
                S.dma("sp", gt_[:], bcast_row(gvec, l * D, D), writes=["gtmp"])
                for s in range(2):
                    sl = MOD[s][:, slot * D:(slot + 1) * D]
                    if kind == "A":
                        S.op("dve", lambda e, sl=sl: e.scalar_tensor_tensor(sl, sl, 1.0, gt_[:], ALU.add, ALU.mult),
                             reads=["gtmp", MODK[s]], writes=[MODK[s]])
                    else:
                        S.op("dve", lambda e, sl=sl: e.tensor_tensor(sl, sl, gt_[:], ALU.mult),
                             reads=["gtmp", MODK[s]], writes=[MODK[s]])
            for s in range(2):
                S.dma("sp", MODD.ap()[s].rearrange("k p d -> p k d"), MOD[s][:].rearrange("p (k d) -> p k d", k=6), reads=[MODK[s]], writes=[("MODD", s)])

        def load_mod(tag, slots, sets=(0, 1)):
            modcur.clear()
            for s in sets:
                t = sb("mc%s_%d" % (tag, s), [128, len(slots), D], F32)
                for i, slot in enumerate(slots):
                    S.dma("sp", t[:, i, :], MODD.ap()[s, slot], writes=["mc%d" % s])
                    modcur[(s, slot)] = (t[:, i, :], "mc%d" % s)

        def modsl(ti, slot):
            s = 1 if ti == NPT else 0
            return modcur[(s, slot)]

        def norm_mod_T(xt, xk, ti, aslot, bslot, dst_ap, dkey, fp32_out=None):
            stt, sk = rms_stats([xt[:]], [xk], D)
            A, ak = modsl(ti, aslot)
            B, bk = modsl(ti, bslot)
            tt, tk = tmpr.next()
            S.op("dve", lambda e: e.scalar_tensor_tensor(tt[:], xt[:], stt[:, 3:4], A, ALU.mult, ALU.mult),
                 reads=[xk, sk, ak], writes=[tk])
            hb, hk = hbr.next()
            if fp32_out is not None:
                fo, fk = fp32_out
                S.op("pool", lambda e: e.tensor_tensor(fo[:], tt[:], B, ALU.add), reads=[tk, bk], writes=[fk])
                S.op("pool", lambda e: e.tensor_copy(hb[:], fo[:]), reads=[fk], writes=[hk])
            else:
                S.op("pool", lambda e: e.tensor_tensor(hb[:], tt[:], B, ALU.add), reads=[tk, bk], writes=[hk])
            transpose8(hb, hk, dst_ap, dkey)

        @phase
        def phase_premix(l, tiles, XIN):
            load_mod("pm%d" % l, (0, 1))
            wres = sb("wresA%d" % l, [128, 8, 2560], BF16)
            for i in range(5):
                load_cast(wres[:, :, i * 512:(i + 1) * 512],
                          w_in.ap()[l][:, i * 512:(i + 1) * 512].rearrange("(kc p) n -> p kc n", p=128), "wres")
            hTr = Ring("hT%d_" % l, 2, [128, 8, 512], BF16)
            qkTr = Ring("qkT%d_" % l, 2, [128, 8, 512], BF16)
            qkr = Ring("qk%d_" % l, 2, [128, D], F32)
            vfr = Ring("vf%d_" % l, 2, [128, 512], F32)
            vbr = Ring("vb%d_" % l, 2, [128, 512], BF16)
            rpr = Ring("rp%d_" % l, 2, [128, 4, 128], F32)
            rtr = Ring("rt%d_" % l, 2, [128, 4, 128], F32)
            sgr = Ring("sg%d_" % l, 2, [128, 512], F32)
            ubr = Ring("ub%d_" % l, 2, [128, 4, 512], BF16)
            uf = sb("uf%d" % l, [128, 4, 128], F32)
            uo = sb("uo%d" % l, [128, 512], F32)
            blocks = []
            i = 0
            while i < len(tiles):
                if tiles[i] == NPT:
                    blocks.append([NPT]); i += 1
                else:
                    blocks.append(tiles[i:i + 4]); i += 4
            for blk in blocks:
                nt = len(blk)
                W = nt * 128
                t0 = blk[0] * 128
                hT, hTk = hTr.next()
                qkT, qkTk = qkTr.next()
                for j, ti in enumerate(blk):
                    xt, xk = xr.next()
                    S.dma("sp", xt[:], XIN.ap()[ti * 128:(ti + 1) * 128, :], reads=[("X", ti)], writes=[xk])
                    norm_mod_T(xt, xk, ti, 1, 0, hT[:, :, j * 128:(j + 1) * 128], hTk)
                    for n3 in range(3):
                        for kc in range(8):
                            S.op("pe", lambda e, n3=n3, kc=kc, j=j, hT=hT: e.matmul(pb[n3][:], hT[:, kc, j * 128:(j + 1) * 128],
                                                                                   wres[:, kc, n3 * 512:(n3 + 1) * 512], start=(kc == 0), stop=(kc == 7)),
                                 reads=[hTk, "wres"], writes=[PB[n3]])
                    qk, qkk = qkr.next()
                    S.op("act", lambda e, qk=qk: e.copy(qk[:, 0:512], pb[0][:]), reads=[PB[0]], writes=[qkk])
                    S.op("act", lambda e, qk=qk: e.copy(qk[:, 512:1024], pb[1][:]), reads=[PB[1]], writes=[qkk])
                    vf, vfk = vfr.next()
                    vb, vbk = vbr.next()
                    S.op("dve", lambda e, vf=vf: e.tensor_copy(vf[:], pb[2][:]), reads=[PB[2]], writes=[vfk])
                    S.op("pool", lambda e, vf=vf, vb=vb: e.tensor_copy(vb[:], vf[:]), reads=[vfk], writes=[vbk])
                    S.dma("sp", VV.ap()[ti * 128:(ti + 1) * 128, :], vb[:], reads=[vbk], writes=[("V", ti)])
                    rp, rpk = rpr.next()
                    S.dma("sp", rp[:], rope.ap()[ti * 128:(ti + 1) * 128], writes=[rpk])
                    rt, rtk = rtr.next()
                    qv = qk[:].rearrange("p (h d) -> p h d", d=64)
                    x1 = qv[:, :, 0:8]
                    x2 = qv[:, :, 8:16]
                    cosv = rp[:, 0, :].rearrange("p (h d) -> p h d", d=8)
                    sinv = rp[:, 1, :].rearrange("p (h d) -> p h d", d=8)
                    tv = [rt[:, i_, :].rearrange("p (h d) -> p h d", d=8) for i_ in range(4)]
                    S.op("dve", lambda e, x1=x1, cosv=cosv, tv=tv: e.tensor_tensor(tv[0], x1, cosv, ALU.mult), reads=[qkk, rpk], writes=[rtk])
                    S.op("dve", lambda e, x2=x2, sinv=sinv, tv=tv: e.tensor_tensor(tv[1], x2, sinv, ALU.mult), reads=[qkk, rpk], writes=[rtk])
                    S.op("dve", lambda e, x2=x2, cosv=cosv, tv=tv: e.tensor_tensor(tv[2], x2, cosv, ALU.mult), reads=[qkk, rpk], writes=[rtk])
                    S.op("dve", lambda e, x1=x1, sinv=sinv, tv=tv: e.tensor_tensor(tv[3], x1, sinv, ALU.mult), reads=[qkk, rpk], writes=[rtk])
                    S.op("dve", lambda e, x1=x1, tv=tv: e.tensor_tensor(x1, tv[0], tv[1], ALU.subtract), reads=[rtk], writes=[qkk])
                    S.op("dve", lambda e, x2=x2, tv=tv: e.tensor_tensor(x2, tv[2], tv[3], ALU.add), reads=[rtk], writes=[qkk])
                    if ti >= 32:
                        orow = (ti - 32) * 128
                        S.dma("sp", o_k.ap()[l][orow:orow + 128, :], qk[:, 512:1024], reads=[qkk])
                        S.dma("sp", o_v.ap()[l][orow:orow + 128, :], vf[:], reads=[vfk])
                    hb, hk = hbr.next()
                    S.op("pool", lambda e, hb=hb, qk=qk: e.tensor_copy(hb[:], qk[:]), reads=[qkk], writes=[hk])
                    transpose8(hb, hk, qkT[:, :, j * 128:(j + 1) * 128], qkTk, eng="dve")
                S.dma("sp", QT.ap()[:, t0:t0 + W].rearrange("(c p) t -> p c t", p=128), qkT[:, 0:4, 0:W], reads=[qkTk], writes=[("QT", t0)])
                S.dma("sp", KT.ap()[:, t0:t0 + W].rearrange("(c p) t -> p c t", p=128), qkT[:, 4:8, 0:W], reads=[qkTk], writes=[("KT", t0)])
                ub, ubk = ubr.next()
                for cc in range(4):
                    for which, pbi in ((0, 3), (1, 4)):
                        col = 1536 + which * 512 + cc * 128
                        for kc in range(8):
                            S.op("pe", lambda e, kc=kc, col=col, pbi=pbi, hT=hT, W=W: e.matmul(pb[pbi][:, 0:W], wres[:, kc, col:col + 128], hT[:, kc, 0:W],
                                                                                          start=(kc == 0), stop=(kc == 7)),
                                 reads=[hTk, "wres"], writes=[PB[pbi]])
                    sg, sgk = sgr.next()
                    S.op("act", lambda e, sg=sg, W=W: e.activation(sg[:, 0:W], pb[4][:, 0:W], AF.Sigmoid), reads=[PB[4]], writes=[sgk])
                    S.op("dve", lambda e, sg=sg, ub=ub, cc=cc, W=W: e.tensor_tensor(ub[:, cc, 0:W], pb[3][:, 0:W], sg[:, 0:W], ALU.mult),
                         reads=[PB[3], sgk], writes=[ubk])
                    if blk[-1] == 47 or blk[0] == NPT:
                        c0 = W - 128
                        S.op("dve", lambda e, sg=sg, cc=cc, c0=c0: e.tensor_tensor(uf[:, cc, :], pb[3][:, c0:c0 + 128], sg[:, c0:c0 + 128], ALU.mult),
                             reads=[PB[3], sgk], writes=["uf"])
                S.dma("sp", UT.ap()[:, t0:t0 + W].rearrange("(c p) t -> p c t", p=128), ub[:, :, 0:W], reads=[ubk], writes=[("UT", t0)])
                if blk[-1] == 47 or blk[0] == NPT:
                    for cc in range(4):
                        S.op("pe", lambda e, cc=cc: e.transpose(pb[5][:, cc * 128:(cc + 1) * 128], uf[:, cc, :], identf[:]),
                             reads=["uf", "identf"], writes=[PB[5]])
                    S.op("act", lambda e: e.copy(uo[:], pb[5][:]), reads=[PB[5]], writes=["uo"])
                    if blk[-1] == 47:
                        S.dma("sp", o_cp.ap()[l], uo[96:128, :], reads=["uo"])
                    else:
                        for s in range(16):
                            S.dma("sp", o_cs.ap()[l][s, 26:30, :], uo[4 * s:4 * s + 4, :], reads=["uo"])
                        S.dma("sp", o_cs.ap()[l][:, 0:26, :], sconv.ap()[l][:, 4:30, :])

        @phase
        def phase_attn(l, q0, maskh, mhk):
            k0 = q0 - 2048
            qhr = Ring("qh%d_%d_" % (l, q0), 2, [128, 2048], BF16)
            khr = Ring("kh%d_%d_" % (l, q0), 2, [128, 4096], BF16)
            vP = {d: sb("vP%d_%d_%d" % (l, q0, d), [128, 16 + d, 128], BF16) for d in (1, 4, 16)}
            Oacc = sb("Oacc%d_%d" % (l, q0), [128, 2048], F32)
            Dacc = sb("Dacc%d_%d" % (l, q0), [128, 2048], F32)
            att = sb("att%d_%d" % (l, q0), [128, 2048], BF16)
            ptr = Ring("pt%d_%d_" % (l, q0), 3, [128, 256], BF16)
            sc_i = [0]
            for h in range(8):
                p0 = 64 * (h % 2)
                pr = slice(p0, p0 + 64)
                qh, qhk = qhr.next()
                kh, khk = khr.next()
                S.dma("sp", qh[pr, :], QT.ap()[64 * h:64 * h + 64, q0:q0 + 2048], writes=[qhk])
                S.dma("sp", kh[pr, :], KT.ap()[64 * h:64 * h + 64, k0:k0 + 4096], writes=[khk])
                if h % 2 == 0:
                    pc = slice(64 * h, 64 * h + 128)
                    for d in (1, 4, 16):
                        nkb = 16 // d + 1
                        kb0 = 16 // d - 1
                        for r in range(d):
                            base = k0 + d * 128 * kb0 + r
                            src = bass.AP(VV, base * 512 + 64 * h, [[d * 512, 128], [128 * d * 512, nkb], [1, 128]])
                            S.dma("sp", vP[d][:, r * nkb:(r + 1) * nkb, :], src, writes=["vP%d" % d])
                first = {1: True, 4: True, 16: True}
                for d in (1, 4, 16):
                    nkb = 16 // d + 1
                    kb0 = 16 // d - 1
                    nqb = 16 // d
                    for r in range(d):
                        for kbi in range(nkb):
                            kb = kb0 + kbi
                            has_same = kbi >= 1
                            has_next = kbi < nkb - 1
                            if has_same and has_next:
                                msel = (0, 256)
                            elif has_next:
                                msel = (128, 256)
                            else:
                                msel = (0, 128)
                            N = msel[1] - msel[0]
                            jb_first = kb if has_same else kb + 1
                            qcol = d * 128 * jb_first + r - 2048
                            kcol = d * 128 * kb + r
                            spb = pb[sc_i[0] % 2]; spk = PB[sc_i[0] % 2]; sc_i[0] += 1
                            mk_ = maskh if kbi == 0 else maskb
                            mkk = mhk if kbi == 0 else "maskb"
                            S.op("pe", lambda e, spb=spb, kcol=kcol, qcol=qcol, N=N, d=d, pr=pr, kh=kh, qh=qh: e.matmul(
                                spb[:, 0:N], kh[pr, kcol:kcol + 127 * d + 1:d], qh[pr, qcol:qcol + (N - 1) * d + 1:d], start=True, stop=False),
                                reads=[khk, qhk], writes=[spk])
                            S.op("pe", lambda e, spb=spb, mk_=mk_, msel=msel, N=N: e.matmul(
                                spb[:, 0:N], ident[:], mk_[:, msel[0]:msel[1]], start=False, stop=True),
                                reads=["ident", mkk], writes=[spk])
                            pt, ptk = ptr.next()
                            S.op("act", lambda e, pt=pt, spb=spb, N=N: e.activation(pt[:, 0:N], spb[:, 0:N], AF.Exp, scale=0.125),
                                 reads=[spk], writes=[ptk])
                            vt = vP[d][:, r * nkb + kbi, :]
                            off = 0
                            for which in ("same", "next"):
                                if which == "same" and not has_same:
                                    continue
                                if which == "next" and not has_next:
                                    continue
                                jb = kb if which == "same" else kb + 1
                                qi = jb - 16 // d
                                gq = r * nqb + qi
                                bank = 2 + (gq // 4) % 2
                                dbank = 4 + (gq // 4) % 2
                                cs = (gq % 4) * 128
                                S.op("pe", lambda e, bank=bank, cs=cs, vt=vt, pt=pt, off=off, which=which: e.matmul(
                                    pb[bank][:, cs:cs + 128], vt, pt[:, off:off + 128], start=(which == "next"), stop=(which == "same")),
                                    reads=["vP%d" % d, ptk], writes=[PB[bank]])
                                S.op("pe", lambda e, dbank=dbank, cs=cs, pt=pt, off=off, which=which: e.matmul(
                                    pb[dbank][:, cs:cs + 128], onesb[:], pt[:, off:off + 128], start=(which == "next"), stop=(which == "same")),
                                    reads=["onesb", ptk], writes=[PB[dbank]])
                                off += 128
                                if which == "same" and gq % 4 == 3:
                                    g4 = gq // 4
                                    for (acc, bk_, akey) in ((Oacc, bank, "Oacc"), (Dacc, dbank, "Dacc")):
                                        if d == 1:
                                            dst = [(acc[pr, g4 * 512:(g4 + 1) * 512], pb[bk_][pr, :])]
                                        elif d == 4:
                                            dst = [(acc[pr, r:2048:4], pb[bk_][pr, :])]
                                        else:
                                            dst = [(acc[pr, (4 * g4 + rr):2048:16], pb[bk_][pr, rr * 128:(rr + 1) * 128]) for rr in range(4)]
                                        for (da, sa) in dst:
                                            if d == 1:
                                                S.op("dve", lambda e, da=da, sa=sa: e.tensor_copy(da, sa), reads=[PB[bk_]], writes=[akey])
                                            else:
                                                S.op("dve", lambda e, da=da, sa=sa: e.tensor_tensor(da, da, sa, ALU.add), reads=[PB[bk_], akey], writes=[akey])
                S.op("dve", lambda e, pr=pr: e.reciprocal(Dacc[pr, :], Dacc[pr, :]), reads=["Dacc"], writes=["Dacc"])
                S.op("dve", lambda e, pr=pr: e.tensor_tensor(att[pr, :], Oacc[pr, :], Dacc[pr, :], ALU.mult), reads=["Oacc", "Dacc"], writes=["att"])
                S.dma("sp", CAT.ap()[64 * h:64 * h + 64, q0:q0 + 2048], att[pr, :], reads=["att"], writes=[("CATa", h, q0)])

        def conv_consts(l):
            cw = sb("cw%d" % l, [128, 4, 31], F32)
            cvec = sb("cvec%d" % l, [128, 3, 4], F32)
            dg = sb("dg%d" % l, [128, 4, 31, 128], BF16)
            S.dma("sp", cw[:], conv_wT.ap()[l].rearrange("(c p) j -> p c j", p=128), writes=["cw"])
            for i, vec in enumerate((conv_b, conv_ln_g, conv_ln_b)):
                S.dma("sp", cvec[:, i, :], bass.AP(vec, l * 512, [[1, 128], [128, 4]]), writes=["cvec"], allow_slow_non_contiguous=True)
            for cc in range(4):
                for j in range(31):
                    S.op("dve", lambda e, cc=cc, j=j: e.tensor_scalar(dg[:, cc, j, :], identf[:], cw[:, cc, j:j + 1], None, ALU.mult),
                         reads=["identf", "cw"], writes=["dg"])
            return cvec, dg

        def conv_block(l, cvec, dg, rhs_fn, W, rkeys, out_cols, rings, oview=lambda a: a):
            cvr, sqr, mnr, cbr, t1r = rings
            cvf, cvk = cvr.next()
            sq, sqk = sqr.next()
            for cc in range(4):
                pc = pb[cc % 2]; pck = PB[cc % 2]
                for j in range(31):
                    rhs = rhs_fn(cc, j)
                    S.op("pe", lambda e, pc=pc, cc=cc, j=j, rhs=rhs: e.matmul(oview(pc[:, 0:W]), dg[:, cc, j, :], rhs, start=(j == 0), stop=(j == 30)),
                         reads=["dg"] + rkeys, writes=[pck])
                S.op("act", lambda e, pc=pc, cc=cc, cvf=cvf: e.activation(cvf[:, cc, 0:W], pc[:, 0:W], AF.Identity, bias=cvec[:, 0, cc:cc + 1]),
                     reads=[pck, "cvec"], writes=[cvk])
                S.op("act", lambda e, cc=cc, cvf=cvf, sq=sq: e.activation(sq[:, cc, 0:W], cvf[:, cc, 0:W], AF.Square), reads=[cvk], writes=[sqk])
            for cc in range(4):
                S.op("pe", lambda e, cc=cc, cvf=cvf: e.matmul(pb[2][:, 0:W], onesf[:], cvf[:, cc, 0:W], start=(cc == 0), stop=(cc == 3)),
                     reads=["onesf", cvk], writes=[PB[2]])
            for cc in range(4):
                S.op("pe", lambda e, cc=cc, sq=sq: e.matmul(pb[3][:, 0:W], onesf[:], sq[:, cc, 0:W], start=(cc == 0), stop=(cc == 3)),
                     reads=["onesf", sqk], writes=[PB[3]])
            mn, mnk = mnr.next()
            S.op("act", lambda e, mn=mn: e.copy(mn[:, 0, 0:W], pb[2][:, 0:W]), reads=[PB[2]], writes=[mnk])
            S.op("pool", lambda e, mn=mn: e.tensor_tensor(mn[:, 1, 0:W], mn[:, 0, 0:W], mn[:, 0, 0:W], ALU.mult), reads=[mnk], writes=[mnk])
            S.op("dve", lambda e, mn=mn: e.tensor_tensor(mn[:, 1, 0:W], pb[3][:, 0:W], mn[:, 1, 0:W], ALU.subtract), reads=[PB[3], mnk], writes=[mnk])
            S.op("dve", lambda e, mn=mn: e.tensor_scalar(mn[:, 1, 0:W], mn[:, 1, 0:W], 0.0, None, ALU.max), reads=[mnk], writes=[mnk])
            S.op("act", lambda e, mn=mn: e.activation(mn[:, 2, 0:W], mn[:, 1, 0:W], AF.Sqrt, bias=epsb[:, 0:1]), reads=[mnk, "epsb"], writes=[mnk])
            S.op("dve", lambda e, mn=mn: e.reciprocal(mn[:, 2, 0:W], mn[:, 2, 0:W]), reads=[mnk], writes=[mnk])
            cb, cbk = cbr.next()
            for cc in range(4):
                t1, t1k = t1r.next()
                S.op("dve", lambda e, cc=cc, t1=t1, cvf=cvf, mn=mn: e.tensor_tensor(t1[:, 0:W], cvf[:, cc, 0:W], mn[:, 0, 0:W], ALU.subtract),
                     reads=[cvk, mnk], writes=[t1k])
                S.op("pool", lambda e, t1=t1, mn=mn: e.tensor_tensor(t1[:, 0:W], t1[:, 0:W], mn[:, 2, 0:W], ALU.mult), reads=[t1k, mnk], writes=[t1k])
                S.op("act", lambda e, cc=cc, t1=t1, cb=cb: e.activation(cb[:, cc, 0:W], t1[:, 0:W], AF.Silu, bias=cvec[:, 2, cc:cc + 1], scale=cvec[:, 1, cc:cc + 1]),
                     reads=[t1k, "cvec"], writes=[cbk])
            S.dma("sp", CAT.ap()[512:1024, out_cols:out_cols + W].rearrange("(c p) t -> p c t", p=128), cb[:, :, 0:W], reads=[cbk],
                  writes=[("CATc", out_cols)])

        @phase
        def phase_conv(l, starts):
            cvec, dg = conv_consts(l)
            rings = (Ring("cvf%d_" % l, 2, [128, 4, 512], F32), Ring("csq%d_" % l, 2, [128, 4, 512], F32),
                     Ring("cmn%d_" % l, 2, [128, 3, 512], F32), Ring("ccb%d_" % l, 2, [128, 4, 512], BF16),
                     Ring("ct1%d_" % l, 2, [128, 512], F32))
            ubr = Ring("cub%d_" % l, 2, [128, 4, 544], BF16)
            for (tok0, fcol) in starts:
                ub, ubk = ubr.next()
                S.dma("sp", ub[:], UT.ap()[:, tok0 - 32:tok0 + 512].rearrange("(c p) t -> p c t", p=128), writes=[ubk])
                if fcol is not None:
                    S.op("dve", lambda e, ub=ub, fcol=fcol: e.tensor_scalar(ub[:, :, 0:32], ub[:, :, 0:32], flg[:, fcol:fcol + 1], None, ALU.mult),
                         reads=[ubk, "flg"], writes=[ubk])
                conv_block(l, cvec, dg, lambda cc, j, ub=ub: ub[:, cc, 2 + j:2 + j + 512], 512, [ubk], tok0, rings)
            usf = sb("usf%d" % l, [128, 4, 16, 34], BF16)
            stt_ = Ring("sst%d_" % l, 2, [120, 512], F32)
            un = sb("sun%d" % l, [128, 4, 128], BF16)
            S.dma("sp", un[:], UT.ap()[:, NPT * 128:NPT * 128 + 128].rearrange("(c p) t -> p c t", p=128), writes=["sun"])
            for g in range(4):
                stile, stk = stt_.next()
                S.dma("sp", stile[:], sconv.ap()[l][4 * g:4 * g + 4].rearrange("s j c -> (s j) c"), writes=[stk])
                for cc in range(4):
                    S.op("pe", lambda e, cc=cc, stile=stile: e.transpose(pb[4][:, cc * 120:(cc + 1) * 120], stile[:, cc * 128:(cc + 1) * 128], identf[0:120, 0:120]),
                         reads=[stk, "identf"], writes=[PB[4]])
                for cc in range(4):
                    S.op("act", lambda e, cc=cc, g=g: e.copy(usf[:, cc, 4 * g:4 * g + 4, 0:30],
                                                             pb[4][:, cc * 120:(cc + 1) * 120].rearrange("p (s j) -> p s j", j=30)),
                         reads=[PB[4]], writes=["usf"])
            for cc in range(4):
                S.op("dve", lambda e, cc=cc: e.tensor_copy(usf[:, cc, :, 30:34], un[:, cc, 0:64].rearrange("p (s t) -> p s t", t=4)),
                     reads=["sun"], writes=["usf"])
            conv_block(l, cvec, dg, lambda cc, j: usf[:, cc, :, j:j + 4], 64, ["usf"], NPT * 128, rings,
                       oview=lambda a: a.rearrange("p (s t) -> p s t", t=4))

        def router(l, ti, h2f, h2k):
            wr = router_w
            hTf = sb_router["hTf"]
            for kc in range(8):
                S.op("pe", lambda e, kc=kc: e.transpose(pb[5][:, (kc % 4) * 128:(kc % 4 + 1) * 128], h2f[:, kc * 128:(kc + 1) * 128], identf[:]),
                     reads=[h2k, "identf"], writes=[PB[5]])
                if kc % 4 == 3:
                    S.op("act", lambda e, kc=kc: e.copy(hTf[:, (kc // 4) * 512:(kc // 4 + 1) * 512], pb[5][:]), reads=[PB[5]], writes=["hTf"])
            for kc in range(8):
                S.op("pe", lambda e, kc=kc: e.matmul(pb[6][:, 0:8], hTf[:, kc * 128:(kc + 1) * 128], wr[:, kc, :], start=(kc == 0), stop=(kc == 7)),
                     reads=["hTf", "wr"], writes=[PB[6]])
            g, gk = sb_router["ring"].next()
            lg = g[:, 0:8]; eq1 = g[:, 8:16]; l2 = g[:, 16:24]; eq2 = g[:, 24:32]; gate = g[:, 32:40]
            m1 = g[:, 40:41]; m2 = g[:, 41:42]; dm = g[:, 42:43]; ex = g[:, 43:44]; w1 = g[:, 44:45]; w2 = g[:, 45:46]
            S.op("dve", lambda e: e.tensor_copy(lg, pb[6][:, 0:8]), reads=[PB[6]], writes=[gk])
            S.op("dve", lambda e: e.tensor_reduce(m1, lg, AX.X, ALU.max), reads=[gk], writes=[gk])
            S.op("dve", lambda e: e.tensor_scalar(eq1, lg, m1, None, ALU.is_equal), reads=[gk], writes=[gk])
            S.op("dve", lambda e: e.scalar_tensor_tensor(l2, eq1, -1e30, lg, ALU.mult, ALU.add), reads=[gk], writes=[gk])
            S.op("dve", lambda e: e.tensor_reduce(m2, l2, AX.X, ALU.max), reads=[gk], writes=[gk])
            S.op("dve", lambda e: e.tensor_scalar(eq2, l2, m2, None, ALU.is_equal), reads=[gk], writes=[gk])
            S.op("dve", lambda e: e.tensor_tensor(dm, m2, m1, ALU.subtract), reads=[gk], writes=[gk])
            S.op("act", lambda e: e.activation(ex, dm, AF.Exp), reads=[gk], writes=[gk])
            S.op("dve", lambda e: e.tensor_scalar(w1, ex, 1.0, None, ALU.add), reads=[gk], writes=[gk])
            S.op("dve", lambda e: e.reciprocal(w1, w1), reads=[gk], writes=[gk])
            S.op("dve", lambda e: e.tensor_tensor(w2, ex, w1, ALU.mult), reads=[gk], writes=[gk])
            S.op("dve", lambda e: e.tensor_scalar(gate, eq1, w1, None, ALU.mult), reads=[gk], writes=[gk])
            S.op("dve", lambda e: e.scalar_tensor_tensor(gate, eq2, w2, gate, ALU.mult, ALU.add), reads=[gk], writes=[gk])
            S.dma("sp", GATES.ap()[ti * 128:(ti + 1) * 128, :], gate, reads=[gk], writes=[("GATES", ti)])

        router_w = None
        sb_router = {}

        @phase
        def phase_postmix(l, tiles, XIN):
            nonlocal router_w
            load_mod("po%d" % l, (2, 3, 4))
            wres = sb("wresB%d" % l, [128, 8, 1024], BF16)
            for i in range(2):
                load_cast(wres[:, :, i * 512:(i + 1) * 512],
                          w_out.ap()[l][:, i * 512:(i + 1) * 512].rearrange("(kc p) n -> p kc n", p=128), "wres")
            catr = Ring("cat%d_" % l, 2, [128, 8, 512], BF16)
            h2Tr = Ring("h2T%d_" % l, 2, [128, 8, 512], BF16)
            x1r = Ring("x1%d_" % l, 2, [128, D], F32)
            h2fr = Ring("h2f%d_" % l, 2, [128, D], F32)
            if l == 1:
                router_w = sb("wr", [128, 8, 8], F32)
                S.dma("sp", router_w[:], moe_w_router.ap()[0].rearrange("(kc p) n -> p kc n", p=128), writes=["wr"])
                sb_router["hTf"] = sb("hTf", [128, 1024], F32)
                sb_router["ring"] = Ring("rg", 2, [128, 64], F32)
            blocks = []
            i = 0
            while i < len(tiles):
                if tiles[i] == NPT:
                    blocks.append([NPT]); i += 1
                else:
                    blocks.append(tiles[i:i + 4]); i += 4
            for blk in blocks:
                W = len(blk) * 128
                t0 = blk[0] * 128
                cat, catk = catr.next()
                S.dma("sp", cat[:, :, 0:W], CAT.ap()[:, t0:t0 + W].rearrange("(c p) t -> p c t", p=128), writes=[catk])
                h2T, h2Tk = h2Tr.next()
                for j, ti in enumerate(blk):
                    for half in range(2):
                        for kc in range(8):
                            S.op("pe", lambda e, half=half, kc=kc, j=j, cat=cat: e.matmul(pb[half][:], cat[:, kc, j * 128:(j + 1) * 128],
                                                                                       wres[:, kc, half * 512:(half + 1) * 512], start=(kc == 0), stop=(kc == 7)),
                                 reads=[catk, "wres"], writes=[PB[half]])
                    xt, xk = xr.next()
                    S.dma("sp", xt[:], XIN.ap()[ti * 128:(ti + 1) * 128, :], writes=[xk])
                    stt, sk = rms_stats([pb[0][:], pb[1][:]], [PB[0], PB[1]], D)
                    G1, g1k = modsl(ti, 2)
                    tt, tk = tmpr.next()
                    for half in range(2):
                        S.op("dve", lambda e, half=half, tt=tt, stt=stt, G1=G1: e.scalar_tensor_tensor(
                            tt[:, half * 512:(half + 1) * 512], pb[half][:], stt[:, 3:4], G1[:, half * 512:(half + 1) * 512], ALU.mult, ALU.mult),
                            reads=[PB[half], sk, g1k], writes=[tk])
                    x1, x1k = x1r.next()
                    S.op("pool", lambda e, x1=x1, tt=tt, xt=xt: e.tensor_tensor(x1[:], tt[:], xt[:], ALU.add), reads=[tk, xk], writes=[x1k])
                    S.dma("sp", XA.ap()[ti * 128:(ti + 1) * 128, :], x1[:], reads=[x1k], writes=[("XA", ti)])
                    if l == 1:
                        h2f, h2k = h2fr.next()
                        norm_mod_T(x1, x1k, ti, 4, 3, h2T[:, :, j * 128:(j + 1) * 128], h2Tk, fp32_out=(h2f, h2k))
                        router(l, ti, h2f, h2k)
                    else:
                        norm_mod_T(x1, x1k, ti, 4, 3, h2T[:, :, j * 128:(j + 1) * 128], h2Tk)
                S.dma("sp", H2T.ap()[:, t0:t0 + W].rearrange("(c p) t -> p c t", p=128), h2T[:, :, 0:W], reads=[h2Tk], writes=[("H2T", t0)])

        @phase
        def phase_ffn(l, gi, blocks, experts, XOUT, out_map):
            Tg = sum(w for _, w in blocks)
            FFD = int(os.environ.get("KDBG_FFN", "3"))
            nm = "f%d_%d" % (l, gi)
            load_mod(nm, (5,))
            H2 = sb(nm + "H2", [128, 8, Tg], BF16)
            facc = sb(nm + "fa", [128, 8, Tg], F32)
            moe = len(experts) > 1
            offs = []
            o = 0
            for (t0, W) in blocks:
                S.dma("sp", H2[:, :, o:o + W], H2T.ap()[:, t0:t0 + W].rearrange("(c p) t -> p c t", p=128), writes=[nm + "H2"])
                offs.append(o); o += W
            if moe:
                GBr = Ring(nm + "GB", 2, [128, Tg], BF16)
                gts = sb(nm + "gts", [128, Tg // 128, 8], F32)
                dgr = Ring(nm + "dgm", 2, [128, 128], F32)
                for bi, (t0, W) in enumerate(blocks):
                    for j in range(W // 128):
                        S.dma("sp", gts[:, offs[bi] // 128 + j, :], GATES.ap()[t0 + j * 128:t0 + (j + 1) * 128, :], writes=[nm + "gts"])
            wgr = Ring(nm + "wg", 2, [128, 8, 512], BF16)
            wur = Ring(nm + "wu", 2, [128, 8, 512], BF16)
            wdr = Ring(nm + "wd", 1, [128, 4, 1024], BF16)
            stgr = Ring(nm + "stg", 3, [128, 2, 512], F32)
            sgr = Ring(nm + "sg", 2, [128, 512], BF16)
            hidr = Ring(nm + "hid", 2, [128, 4, 512], BF16)
            first = True
            pgi = [0]
            for ex, (wg_ap, wu_ap, wd_ap) in enumerate(experts):
                if moe:
                    GB, gbk = GBr.next()
                    for jt in range(Tg // 128):
                        dgm, dgk = dgr.next()
                        S.op("dve", lambda e, dgm=dgm, jt=jt, ex=ex: e.tensor_scalar(dgm[:], identf[:], gts[:, jt, ex:ex + 1], None, ALU.mult),
                             reads=["identf", nm + "gts"], writes=[dgk])
                        S.op("pe", lambda e, dgm=dgm: e.matmul(pb[6][:, 0:128], ones1[:], dgm[:], start=True, stop=True),
                             reads=["ones1", dgk], writes=[PB[6]])
                        S.op("act", lambda e, GB=GB, jt=jt: e.copy(GB[:, jt * 128:(jt + 1) * 128], pb[6][:, 0:128]), reads=[PB[6]], writes=[gbk])
                for hg in range(7):
                    wg, wgk = wgr.next(); wu, wuk = wur.next(); wd, wdk = wdr.next()
                    for q4 in range(4):
                        for (wt_, wk_, src_) in ((wg, wgk, wg_ap), (wu, wuk, wu_ap)):
                            stg, stgk = stgr.next()
                            S.dma("sp", stg[:], src_[q4 * 256:(q4 + 1) * 256, hg * 512:(hg + 1) * 512].rearrange("(kc p) n -> p kc n", p=128), writes=[stgk])
                            S.op("pool", lambda e, wt_=wt_, stg=stg, q4=q4: e.tensor_copy(wt_[:, 2 * q4:2 * q4 + 2, :], stg[:]), reads=[stgk], writes=[wk_])
                    for q4 in range(4):
                        stg, stgk = stgr.next()
                        S.dma("sp", stg[:], wd_ap[hg * 512 + (q4 // 2) * 256:hg * 512 + (q4 // 2 + 1) * 256, (q4 % 2) * 512:(q4 % 2 + 1) * 512].rearrange("(kc p) n -> p kc n", p=128), writes=[stgk])
                        S.op("pool", lambda e, wd=wd, stg=stg, q4=q4: e.tensor_copy(wd[:, 2 * (q4 // 2):2 * (q4 // 2) + 2, (q4 % 2) * 512:(q4 % 2 + 1) * 512], stg[:]), reads=[stgk], writes=[wdk])
                    for bi, (t0, W) in enumerate(blocks):
                        o = offs[bi]
                        hid, hidk = hidr.next()
                        for mc in range(4):
                            pg = pb[0 + pgi[0] % 2]; pgk = PB[0 + pgi[0] % 2]
                            pu = pb[2 + pgi[0] % 2]; puk = PB[2 + pgi[0] % 2]
                            pgi[0] += 1
                            for kc in range(8):
                                S.op("pe", lambda e, pg=pg, wg=wg, kc=kc, mc=mc, o=o, W=W: e.matmul(pg[:, 0:W], wg[:, kc, mc * 128:(mc + 1) * 128], H2[:, kc, o:o + W],
                                                                                                  start=(kc == 0), stop=(kc == 7)),
                                     reads=[wgk, nm + "H2"], writes=[pgk])
                            for kc in range(8):
                                S.op("pe", lambda e, pu=pu, wu=wu, kc=kc, mc=mc, o=o, W=W: e.matmul(pu[:, 0:W], wu[:, kc, mc * 128:(mc + 1) * 128], H2[:, kc, o:o + W],
                                                                                                  start=(kc == 0), stop=(kc == 7)),
                                     reads=[wuk, nm + "H2"], writes=[puk])
                            sg, sgk = sgr.next()
                            S.op("act", lambda e, sg=sg, pg=pg, W=W: e.activation(sg[:, 0:W], pg[:, 0:W], AF.Silu), reads=[pgk], writes=[sgk])
                            if moe:
                                S.op("pool", lambda e, sg=sg, GB=GB, o=o, W=W: e.tensor_tensor(sg[:, 0:W], sg[:, 0:W], GB[:, o:o + W], ALU.mult),
                                     reads=[sgk, gbk], writes=[sgk])
                            S.op("dve", lambda e, hid=hid, mc=mc, pu=pu, sg=sg, W=W: e.tensor_tensor(hid[:, mc, 0:W], pu[:, 0:W], sg[:, 0:W], ALU.mult),
                                 reads=[puk, sgk], writes=[hidk])
                        for fc in range(8 if FFD >= 2 else 0):
                            pd = pb[4 + fc % 2]; pdk = PB[4 + fc % 2]
                            for mc in range(4):
                                S.op("pe", lambda e, pd=pd, wd=wd, mc=mc, fc=fc, hid=hid, W=W: e.matmul(pd[:, 0:W], wd[:, mc, fc * 128:(fc + 1) * 128], hid[:, mc, 0:W],
                                                                                                      start=(mc == 0), stop=(mc == 3)),
                                     reads=[wdk, hidk], writes=[pdk])
                            fk = (nm + "fa", bi, fc)
                            if first:
                                S.op("dve", lambda e, pd=pd, fc=fc, o=o, W=W: e.tensor_copy(facc[:, fc, o:o + W], pd[:, 0:W]), reads=[pdk], writes=[fk])
                            else:
                                S.op("dve", lambda e, pd=pd, fc=fc, o=o, W=W: e.tensor_tensor(facc[:, fc, o:o + W], facc[:, fc, o:o + W], pd[:, 0:W], ALU.add),
                                     reads=[pdk, fk], writes=[fk])
                    first = False
            if FFD < 3:
                return
            for bi, (t0, W) in enumerate(blocks):
                for j in range(W // 128):
                    ti = t0 // 128 + j
                    c0 = offs[bi] + j * 128
                    for fc in range(8):
                        S.op("pe", lambda e, fc=fc, c0=c0: e.transpose(pb[fc // 4][:, (fc % 4) * 128:(fc % 4 + 1) * 128], facc[:, fc, c0:c0 + 128], identf[:]),
                             reads=[(nm + "fa", bi, fc), "identf"], writes=[PB[fc // 4]])
                    xt, xk = xr.next()
                    S.dma("sp", xt[:], XA.ap()[ti * 128:(ti + 1) * 128, :], writes=[xk])
                    stt, sk = rms_stats([pb[0][:], pb[1][:]], [PB[0], PB[1]], D)
                    G2, g2k = modsl(ti, 5)
                    tt, tk = tmpr.next()
                    for half in range(2):
                        S.op("dve", lambda e, half=half, tt=tt, stt=stt, G2=G2: e.scalar_tensor_tensor(
                            tt[:, half * 512:(half + 1) * 512], pb[half][:], stt[:, 3:4], G2[:, half * 512:(half + 1) * 512], ALU.mult, ALU.mult),
                            reads=[PB[half], sk, g2k], writes=[tk])
                    x2, x2k = tt, tk
                    S.op("pool", lambda e, tt=tt, xt=xt: e.tensor_tensor(tt[:], tt[:], xt[:], ALU.add), reads=[tk, xk], writes=[tk])
                    dst = out_map(ti)
                    if dst is not None:
                        S.dma("sp", dst, x2[:], reads=[x2k])

        @phase
        def phase_sattn(l):
            nm = "sa%d" % l
            qT = sb(nm + "qT", [128, 4, 128], BF16)
            kT = sb(nm + "kT", [128, 4, 128], BF16)
            S.dma("sp", qT[:], QT.ap()[:, NPT * 128:NPT * 128 + 128].rearrange("(c p) t -> p c t", p=128), writes=[nm + "qT"])
            S.dma("sp", kT[:], KT.ap()[:, NPT * 128:NPT * 128 + 128].rearrange("(c p) t -> p c t", p=128), writes=[nm + "kT"])
            kfr = Ring(nm + "kf", 3, [128, 512], F32)
            kbr = Ring(nm + "kb", 3, [128, 512], BF16)
            kTr = Ring(nm + "kTt", 3, [128, 4, 128], BF16)
            vbr = Ring(nm + "vb", 18, [128, 512], BF16)
            vn = sb(nm + "vn", [128, 512], BF16)
            S.dma("sp", vn[:], VV.ap()[NPT * 128:NPT * 128 + 128, :], writes=[nm + "vn"])
            pnr = Ring(nm + "pn", 2, [128, 32], BF16)
            ptr = Ring(nm + "pt", 2, [128, 288], BF16)
            pf = sb(nm + "pf", [128, 320], F32)
            Osb = sb(nm + "O", [128, 32], F32)
            Dsb = sb(nm + "D", [128, 32], F32)
            attS = sb(nm + "att", [128, 4, 128], BF16)
            S.op("pool", lambda e: e.memset(attS[:], 0.0), writes=[nm + "att"])
            tl = [(1920, 1)] + [(1536 + r, 4) for r in range(4)] + [(r, 16) for r in range(4)]
            SA = int(os.environ.get("KDBG_SA", "99"))
            for s in range(16 if SA >= 99 else 1):
                vts = []
                for ti_, (row0, stride) in enumerate(tl):
                    kf, kfk = kfr.next()
                    S.dma("sp", kf[:], bass.AP(ck, ((l * 16 + s) * 2048 + row0) * 512, [[stride * 512, 128], [1, 512]]), writes=[kfk])
                    kb, kbk = kbr.next()
                    S.op("pool", lambda e, kb=kb, kf=kf: e.tensor_copy(kb[:], kf[:]), reads=[kfk], writes=[kbk])
                    vb, vbk = vbr.next()
                    if SA >= 2:
                        load_cast(vb[:], bass.AP(cv, ((l * 16 + s) * 2048 + row0) * 512, [[stride * 512, 128], [1, 512]]), vbk)
                    vts.append((vb, vbk))
                    if SA < 3:
                        continue
                    for c in range(4):
                        S.op("pe", lambda e, c=c, kb=kb: e.transpose(pT[:, c * 128:(c + 1) * 128], kb[:, c * 128:(c + 1) * 128], ident[:]),
                             reads=[kbk, "ident"], writes=["pT"])
                    kTt, kTk = kTr.next()
                    S.op("dve", lambda e, kTt=kTt: e.tensor_copy(kTt[:], pT[:, 0:512].rearrange("p (c t) -> p c t", c=4)), reads=["pT"], writes=[kTk])
                    for h in range(8):
                        p0 = 64 * (h % 2); pr = slice(p0, p0 + 64)
                        oc = h * 36 + ti_ * 4
                        S.op("pe", lambda e, h=h, pr=pr, oc=oc, kTt=kTt, s=s: e.matmul(pb[0][:, oc:oc + 4], kTt[pr, h // 2, :], qT[pr, h // 2, 4 * s:4 * s + 4], start=True, stop=True),
                             reads=[kTk, nm + "qT"], writes=[PB[0]])
                if SA < 4:
                    continue
                for h in range(8):
                    p0 = 64 * (h % 2); pr = slice(p0, p0 + 64)
                    S.op("pe", lambda e, h=h, pr=pr, s=s: e.matmul(pb[1][:, h * 4:h * 4 + 4], kT[pr, h // 2, :], qT[pr, h // 2, 4 * s:4 * s + 4], start=True, stop=True),
                         reads=[nm + "kT", nm + "qT"], writes=[PB[1]])
                if SA < 5:
                    continue
                pt, ptk = ptr.next()
                S.op("act", lambda e: e.activation(pf[:, 0:288], pb[0][:, 0:288], AF.Exp, scale=0.125), reads=[PB[0]], writes=[nm + "pf"])
                S.op("act", lambda e: e.activation(pf[:, 288:320], pb[1][:, 0:32], AF.Exp, scale=0.125), reads=[PB[1]], writes=[nm + "pf"])
                S.op("dve", lambda e, pt=pt: e.tensor_tensor(pt[:], pf[:, 0:288], smask[:, 0:288], ALU.mult), reads=[nm + "pf", "smask"], writes=[ptk])
                pn, pnk = pnr.next()
                S.op("dve", lambda e, pn=pn, s=s: e.tensor_tensor(pn[:], pf[:, 288:320], smask[:, 288 + 32 * s:320 + 32 * s], ALU.mult), reads=[nm + "pf", "smask"], writes=[pnk])
                if SA < 6:
                    continue
                vnn, vnk = vn, nm + "vn"
                for h in range(8):
                    hp = h // 2
                    oc = h * 4
                    for ti_ in range(9):
                        vb, vbk = vts[ti_]
                        pc = h * 36 + ti_ * 4
                        S.op("pe", lambda e, vb=vb, hp=hp, oc=oc, pc=pc, pt=pt, ti_=ti_: e.matmul(pb[2][:, oc:oc + 4], vb[:, hp * 128:(hp + 1) * 128], pt[:, pc:pc + 4], start=(ti_ == 0), stop=False),
                             reads=[vbk, ptk], writes=[PB[2]])
                        S.op("pe", lambda e, oc=oc, pc=pc, pt=pt, ti_=ti_: e.matmul(pb[3][:, oc:oc + 4], onesb[:], pt[:, pc:pc + 4], start=(ti_ == 0), stop=False),
                             reads=["onesb", ptk], writes=[PB[3]])
                    S.op("pe", lambda e, hp=hp, oc=oc, h=h, vnn=vnn, pn=pn: e.matmul(pb[2][:, oc:oc + 4], vnn[:, hp * 128:(hp + 1) * 128], pn[:, h * 4:h * 4 + 4], start=False, stop=True),
                         reads=[vnk, pnk], writes=[PB[2]])
                    S.op("pe", lambda e, oc=oc, h=h, pn=pn: e.matmul(pb[3][:, oc:oc + 4], onesb[:], pn[:, h * 4:h * 4 + 4], start=False, stop=True),
                         reads=["onesb", pnk], writes=[PB[3]])
                if SA < 7:
                    continue
                S.op("dve", lambda e: e.reciprocal(Dsb[:], pb[3][:, 0:32]), reads=[PB[3]], writes=[nm + "D"])
                S.op("dve", lambda e: e.tensor_tensor(Osb[:], pb[2][:, 0:32], Dsb[:], ALU.mult), reads=[PB[2], nm + "D"], writes=[nm + "O"])
                for h in range(8):
                    p0 = 64 * (h % 2); pr = slice(p0, p0 + 64)
                    S.op("pool", lambda e, h=h, pr=pr, s=s: e.tensor_copy(attS[pr, h // 2, 4 * s:4 * s + 4], Osb[pr, h * 4:h * 4 + 4]),
                         reads=[nm + "O"], writes=[nm + "att"])
            S.dma("sp", CAT.ap()[0:512, NPT * 128:NPT * 128 + 128].rearrange("(c p) t -> p c t", p=128), attS[:], reads=[nm + "att"], writes=[("CATa", "s")])

        Bt = list(range(16, 32)); Ct = list(range(32, 48)); At = list(range(0, 16))
        XIN0 = xin

        def ymap(ti):
            if ti >= 32:
                r = (ti - 32) * 128
                return o_y.ap()[r:r + 128, :]
            return None

        def xbmap(ti):
            return XB.ap()[ti * 128:(ti + 1) * 128, :]

        def blocks_of(tiles):
            out = []
            i = 0
            while i < len(tiles):
                if tiles[i] == NPT:
                    out.append((NPT * 128, 128)); i += 1
                else:
                    out.append((tiles[i] * 128, 512)); i += 4
            return out

        phase_mod(0)
        phase_premix(0, At + Bt + Ct + [NPT], xin)
        phase_attn(0, 2048, maskhB, "maskhB")
        phase_attn(0, 4096, maskhC, "maskhC")
        phase_sattn(0)
        phase_conv(0, [(2048 + 512 * i, 2 if i == 0 else None) for i in range(4)] + [(4096 + 512 * i, 3 if i == 0 else None) for i in range(4)])
        phase_postmix(0, Bt + Ct + [NPT], xin)
        dense = [(ffn_w_gate.ap()[0], ffn_w_up.ap()[0], ffn_w_down.ap()[0])]
        phase_ffn(0, 0, blocks_of(Bt[:8]), dense, XB, xbmap)
        phase_ffn(0, 1, blocks_of(Bt[8:]), dense, XB, xbmap)
        phase_ffn(0, 2, blocks_of(Ct[:8]), dense, XB, xbmap)
        last9 = [(40 * 128, 384), (43 * 128, 384), (46 * 128, 384)]
        phase_ffn(0, 3, last9, dense, XB, xbmap)
        phase_mod(1)
        phase_premix(1, Bt + Ct + [NPT], XB)
        phase_attn(1, 4096, maskhC, "maskhC")
        phase_sattn(1)
        phase_conv(1, [(4096 + 512 * i, 3 if i == 0 else None) for i in range(4)])
        phase_postmix(1, Ct + [NPT], XB)
        moe = [(moe_w_gate.ap()[0][e], moe_w_up.ap()[0][e], moe_w_down.ap()[0][e]) for e in range(8)]
        phase_ffn(1, 0, blocks_of(Ct[:8]), moe, None, ymap)
        phase_ffn(1, 1, last9, moe, None, ymap)
        S.final_wait("sp")
        S.flush()
        _DBG["cnt"] = dict(S.cnt)
        _DBG["umax"] = tuple(umax)
        _DBG["slots"] = {q: max(v) for q, v in S.slot_uses.items()}
    return nc


_NC_CACHE = {}
_DBG = {}


def _host_consts():
    m = np.arange(128)[:, None]
    a = np.arange(128)[None, :]
    lower = np.where(m <= a, 0.0, NEG)
    upper = np.where(m >= a, 0.0, NEG)
    mask = np.zeros((128, 1056), np.float32)
    mask[:, 0:128] = lower
    mask[:, 128:256] = upper
    sm = np.zeros((128, 288), np.float32)
    for h in range(8):
        for t in range(4):
            sm[:, h * 36 + t] = (np.arange(128) >= t).astype(np.float32)
            for pat in range(2):
                sm[:, h * 36 + 4 * (1 + 4 * pat + t) + t] = 1.0
    mask[:, 256:544] = sm
    nk = np.zeros((4, 4), np.float32)
    for tp in range(4):
        for t in range(4):
            nk[tp, t] = 1.0 if tp < t else (3.0 if tp == t else 0.0)
    for s_ in range(16):
        mask[4 * s_:4 * s_ + 4, 544 + 32 * s_:544 + 32 * s_ + 32] = np.tile(nk, (1, 8))
    return mask


def _rope_tables(pos):
    half = 8
    inv_freq = (np.float32(500000.0) ** (-np.arange(half, dtype=np.float32) * np.float32(2.0) / np.float32(16))).astype(np.float32)
    ang = pos.astype(np.float32)[:, None] * inv_freq[None, :]
    cos = np.cos(ang).astype(np.float32)
    sin = np.sin(ang).astype(np.float32)
    out = np.zeros((pos.shape[0], 4, 128), np.float32)
    out[:, 0, :] = np.tile(cos, (1, 16))
    out[:, 1, :] = np.tile(sin, (1, 16))
    return out


def kernel(x_prompt, x_sample, cache_k, cache_v, state_conv, c_prompt, c_sample,
           w_mod, b_mod, g_pre_mix, g_post_mix, g_pre_ffn, g_post_ffn, w_in,
           conv_w, conv_b, conv_ln_g, conv_ln_b, w_out,
           ffn_w_gate, ffn_w_up, ffn_w_down,
           moe_w_router, moe_w_gate, moe_w_up, moe_w_down):
    f = lambda a: np.ascontiguousarray(np.asarray(a, dtype=np.float32))
    x_prompt, x_sample, cache_k, cache_v, state_conv = map(f, (x_prompt, x_sample, cache_k, cache_v, state_conv))
    c_prompt, c_sample = f(c_prompt), f(c_sample)
    import os
    if os.environ.get("KDBG_ZS"):
        x_sample = x_sample * 0
    if "nc" not in _NC_CACHE:
        _NC_CACHE["nc"] = build_nc()
    nc = _NC_CACHE["nc"]
    mask = _host_consts()
    shared = {
        "w_mod": f(w_mod), "b_mod": f(b_mod), "g_pre_mix": f(g_pre_mix), "g_post_mix": f(g_post_mix),
        "g_pre_ffn": f(g_pre_ffn), "g_post_ffn": f(g_post_ffn), "w_in": f(w_in),
        "conv_wT": f(np.transpose(np.asarray(conv_w), (0, 2, 1))), "conv_b": f(conv_b),
        "conv_ln_g": f(conv_ln_g), "conv_ln_b": f(conv_ln_b), "w_out": f(w_out),
        "ffn_w_gate": f(ffn_w_gate), "ffn_w_up": f(ffn_w_up), "ffn_w_down": f(ffn_w_down),
        "moe_w_router": f(moe_w_router), "moe_w_gate": f(moe_w_gate), "moe_w_up": f(moe_w_up),
        "moe_w_down": f(moe_w_down), "maskin": mask,
    }
    import os
    cores = [int(c_) for c_ in os.environ.get("KDBG_CORES", "0,1,2,3,4,5,6,7").split(",")]
    in_maps = []
    for core in cores:
        b, c = core // 4, core % 4
        xin = np.zeros((TALL, D), np.float32)
        for j, cc in enumerate((c - 2, c - 1, c)):
            if cc >= 0:
                xin[j * 2048:(j + 1) * 2048] = x_prompt[b, cc * 2048:(cc + 1) * 2048]
        xin[NPT * 128:NPT * 128 + 64] = x_sample[core * 16:(core + 1) * 16].reshape(64, D)
        cexp = np.zeros((2, 128, D), np.float32)
        cexp[0, :, :] = c_prompt[b][None, :]
        cexp[1, 0:64, :] = np.repeat(c_sample[core * 16:(core + 1) * 16], 4, axis=0)
        flags = np.zeros((128, 4), np.float32)
        flags[:, 0] = 0.0 if c >= 2 else NEG
        flags[:, 1] = 0.0 if c >= 1 else NEG
        flags[:, 2] = 1.0 if c >= 2 else 0.0
        flags[:, 3] = 1.0 if c >= 1 else 0.0
        pos = np.zeros((TALL,), np.int64)
        pos[0:NPT * 128] = np.arange(NPT * 128) + 2048 * (c - 2)
        pos[NPT * 128:] = 2048 + (np.arange(128) % 4)
        m = dict(shared)
        m.update({
            "xin": xin, "cexp": cexp,
            "ck": np.ascontiguousarray(cache_k[:, core * 16:(core + 1) * 16].reshape(2, 16, 2048, 512)),
            "cv": np.ascontiguousarray(cache_v[:, core * 16:(core + 1) * 16].reshape(2, 16, 2048, 512)),
            "sconv": np.ascontiguousarray(state_conv[:, core * 16:(core + 1) * 16]),
            "flags": flags, "rope": _rope_tables(pos),
        })
        in_maps.append(m)
    if os.environ.get("KDBG_TRACE"):
        res = run_bass_kernel_spmd(nc, in_maps, core_ids=list(range(len(cores))), trace=True)
        print("EXEC_TIME_NS", res.exec_time_ns)
    else:
        res = run_bass_kernel_spmd(nc, in_maps, core_ids=list(range(len(cores))))
    R = dict(zip(cores, res.results))
    y_prompt = np.zeros((2, 8192, D), np.float32)
    y_sample = np.zeros((128, 4, D), np.float32)
    nkp = np.zeros((2, 2, 2048, 8, 64), np.float32); nvp = np.zeros_like(nkp)
    ncp = np.zeros((2, 2, 30, 512), np.float32)
    nks = np.zeros((2, 128, 4, 8, 64), np.float32); nvs = np.zeros_like(nks)
    ncs = np.zeros((2, 128, 30, 512), np.float32)
    for core in cores:
        b, c = core // 4, core % 4
        r = R[core]
        y_prompt[b, c * 2048:(c + 1) * 2048] = r["o_y"][0:2048]
        y_sample[core * 16:(core + 1) * 16] = r["o_y"][2048:2048 + 64].reshape(16, 4, D)
        nks[:, core * 16:(core + 1) * 16] = r["o_k"][:, 2048:2048 + 64].reshape(2, 16, 4, 8, 64)
        nvs[:, core * 16:(core + 1) * 16] = r["o_v"][:, 2048:2048 + 64].reshape(2, 16, 4, 8, 64)
        ncs[:, core * 16:(core + 1) * 16] = r["o_cs"]
        if c == 3:
            nkp[:, b] = r["o_k"][:, 0:2048].reshape(2, 2048, 8, 64)
            nvp[:, b] = r["o_v"][:, 0:2048].reshape(2, 2048, 8, 64)
            ncp[:, b] = r["o_cp"][:, 2:32]
    return (y_prompt, y_sample, nkp, nvp, ncp, nks, nvs, ncs)
```

```python
import numpy as np
from contextlib import ExitStack
import ml_dtypes
import concourse.bass as bass
import concourse.mybir as mybir
from concourse.bass_utils import run_bass_kernel_spmd

F32 = mybir.dt.float32
BF16 = mybir.dt.bfloat16
AF = mybir.ActivationFunctionType
ALU = mybir.AluOpType
AX = mybir.AxisListType

COMPUTE = ("pe", "act", "dve", "pool")
NSLOT = 6


class Sched:
    def __init__(self, nc, stack):
        self.nc = nc
        self.eng = {"pe": nc.tensor, "act": nc.scalar, "dve": nc.vector, "pool": nc.gpsimd, "sp": nc.sync}
        self.ops = {e: [] for e in self.eng}
        self.tl = {e: stack.enter_context(nc.semaphore("tl_" + e)) for e in COMPUTE}
        self.cnt = {e: 0 for e in COMPUTE}
        self.slots = {q: [stack.enter_context(nc.semaphore(f"dq_{q}_{i}")) for i in range(NSLOT)]
                      for q in ("sp", "pool", "act")}
        self.slot_uses = {q: [0] * NSLOT for q in self.slots}
        self.slot_next = {q: 0 for q in self.slots}
        self.last_w = {}
        self.reads = {}
        self.seen = {e: {} for e in self.eng}

    def _need(self, e, ev, waits, force=False):
        if ev is None:
            return
        kind, ident, val = ev
        if (not force) and kind == "tl" and ident == e and e == "pe":
            return
        key = (kind, ident)
        if self.seen[e].get(key, 0) >= val:
            return
        self.seen[e][key] = val
        waits[key] = max(waits.get(key, 0), val)

    def _deps(self, e, reads, writes):
        waits = {}
        for k in reads:
            for ev in self.last_w.get(k, ()):
                self._need(e, ev, waits)
        for k in writes:
            for ev in self.last_w.get(k, ()):
                self._need(e, ev, waits)
            for ev in self.reads.get(k, ()):
                self._need(e, ev, waits)
        return waits

    def _commit(self, evs, reads, writes):
        for k in reads:
            self.reads.setdefault(k, []).extend(evs)
        for k in writes:
            self.last_w[k] = list(evs)
            self.reads[k] = []

    def op(self, e, fn, reads=(), writes=()):
        waits = self._deps(e, reads, writes)
        self.cnt[e] += 1
        ev = ("tl", e, self.cnt[e])
        self.ops[e].append((waits, fn, ("tl", e)))
        self._commit([ev], reads, writes)
        return ev

    def dma(self, q, out, in_, reads=(), writes=(), **kw):
        waits = self._deps(q, reads, writes)
        osh = tuple(out.shape)
        parts = []
        if len(osh) == 3 and tuple(in_.shape) == osh and osh[0] * osh[1] > 256 and osh[1] > 1:
            step = max(1, 256 // osh[0])
            for a0 in range(0, osh[1], step):
                a1 = min(osh[1], a0 + step)
                parts.append((out[:, a0:a1, :], in_[:, a0:a1, :]))
        else:
            parts.append((out, in_))
        evs = []
        for i, (o_, i_) in enumerate(parts):
            evs.append(self._dma1(q, o_, i_, waits if i == 0 else {}, **kw))
        self._commit(evs, reads, writes)
        return evs[-1]

    def _dma1(self, q, out, in_, waits, **kw):
        s = self.slot_next[q]
        self.slot_next[q] = (s + 1) % NSLOT
        waits = dict(waits)
        if self.slot_uses[q][s] > 0:
            self._need(q, ("dma", (q, s), 16 * self.slot_uses[q][s]), waits)
        self.slot_uses[q][s] += 1
        ev = ("dma", (q, s), 16 * self.slot_uses[q][s])
        fn = lambda eng, out=out, in_=in_, kw=kw: eng.dma_start(out=out, in_=in_, **kw)
        self.ops[q].append((waits, fn, ("dma", (q, s))))
        return ev

    def _all_events(self):
        evs = [("tl", e, self.cnt[e]) for e in COMPUTE if self.cnt[e] > 0]
        for q in self.slots:
            for s in range(NSLOT):
                if self.slot_uses[q][s] > 0:
                    evs.append(("dma", (q, s), 16 * self.slot_uses[q][s]))
        return evs

    def barrier(self):
        evs = self._all_events()
        for e in self.eng:
            waits = {}
            for ev in evs:
                self._need(e, ev, waits, force=True)
            if waits:
                self.ops[e].append((waits, None, None))
        self.last_w = {}
        self.reads = {}

    def final_wait(self, e="sp"):
        waits = {}
        for ev in self._all_events():
            self._need(e, ev, waits, force=True)
        self.ops[e].append((waits, None, None))

    def _sem(self, key):
        kind, ident = key
        if kind == "tl":
            return self.tl[ident]
        q, s = ident
        return self.slots[q][s]

    def flush(self):
        nc = self.nc
        ops = self.ops
        self.ops = {e: [] for e in self.eng}
        with nc.Block() as block:
            def mk(ename):
                def body(eng):
                    for waits, fn, sig in ops[ename]:
                        for key, val in waits.items():
                            eng.wait_ge(self._sem(key), val)
                        if fn is None:
                            continue
                        inst = fn(eng)
                        if sig[0] == "tl":
                            inst.then_inc(self.tl[sig[1]], 1)
                        else:
                            inst.then_inc(self._sem(sig), 16)
                return body
            block.tensor(mk("pe"))
            block.scalar(mk("act"))
            block.vector(mk("dve"))
            block.gpsimd(mk("pool"))
            block.sync(mk("sp"))


D = 1024
NPT = 48
NT = 49
TALL = NT * 128
DFF = 3584
NEG = -30000.0
EPS = 1e-6


def build_nc():
    nc = bass.Bass("TRN2", target_bir_lowering=False)

    def din(name, shape, dt=F32):
        return nc.dram_tensor(name, list(shape), dt, kind="ExternalInput")

    def dout(name, shape, dt=F32):
        return nc.dram_tensor(name, list(shape), dt, kind="ExternalOutput")

    def dscr(name, shape, dt):
        return nc.dram_tensor(name, list(shape), dt, kind="Internal")

    xin = din("xin", [TALL, D])
    cexp = din("cexp", [2, 128, D])
    ck = din("ck", [2, 16, 2048, 512])
    cv = din("cv", [2, 16, 2048, 512])
    sconv = din("sconv", [2, 16, 30, 512])
    flags = din("flags", [128, 4])
    rope = din("rope", [TALL, 4, 128])
    maskin = din("maskin", [128, 1056])
    w_mod = din("w_mod", [2, D, 6 * D]); b_mod = din("b_mod", [2, 6 * D])
    g_pre_mix = din("g_pre_mix", [2, D]); g_post_mix = din("g_post_mix", [2, D])
    g_pre_ffn = din("g_pre_ffn", [2, D]); g_post_ffn = din("g_post_ffn", [2, D])
    w_in = din("w_in", [2, D, 2560])
    conv_wT = din("conv_wT", [2, 512, 31]); conv_b = din("conv_b", [2, 512])
    conv_ln_g = din("conv_ln_g", [2, 512]); conv_ln_b = din("conv_ln_b", [2, 512])
    w_out = din("w_out", [2, D, D])
    ffn_w_gate = din("ffn_w_gate", [1, D, DFF]); ffn_w_up = din("ffn_w_up", [1, D, DFF])
    ffn_w_down = din("ffn_w_down", [1, DFF, D])
    moe_w_router = din("moe_w_router", [1, D, 8])
    moe_w_gate = din("moe_w_gate", [1, 8, D, DFF]); moe_w_up = din("moe_w_up", [1, 8, D, DFF])
    moe_w_down = din("moe_w_down", [1, 8, DFF, D])
    o_y = dout("o_y", [2048 + 128, D])
    o_k = dout("o_k", [2, 2048 + 128, 512]); o_v = dout("o_v", [2, 2048 + 128, 512])
    o_cp = dout("o_cp", [2, 32, 512]); o_cs = dout("o_cs", [2, 16, 30, 512])
    XA = dscr("XA", [TALL, D], F32); XB = dscr("XB", [TALL, D], F32)
    QT = dscr("QT", [512, TALL], BF16); KT = dscr("KT", [512, TALL], BF16)
    VV = dscr("VV", [TALL, 512], BF16)
    UT = dscr("UT", [512, TALL], BF16)
    CAT = dscr("CAT", [1024, TALL], BF16)
    H2T = dscr("H2T", [1024, TALL], BF16)
    GATES = dscr("GATES", [TALL, 8], F32)
    MODD = dscr("MODD", [2, 6, 128, D], F32)

    st = ExitStack()
    with st:
        S = Sched(nc, st)

        cur = [st]

        usage = [0]
        umax = [0, ""]

        def sb(name, shape, dt):
            n = 1
            for d_ in shape[1:]:
                n *= d_
            n *= 2 if dt == BF16 else 4
            n = (n + 31) // 32 * 32
            usage[-1] += n
            tot = sum(usage)
            if tot > umax[0]:
                umax[0] = tot; umax[1] = name
            _DBG.setdefault("usage", {})[name] = tot
            return cur[-1].enter_context(nc.sbuf_tensor(name, list(shape), dt))

        def psum(name, shape, dt):
            return cur[-1].enter_context(nc.psum_tensor(name, list(shape), dt))

        import os
        nph = [0]
        maxph = int(os.environ.get("KDBG_NPHASE", "999"))

        def phase(fn):
            def wrapped(*a, **k):
                nph[0] += 1
                if nph[0] > maxph:
                    return
                S.barrier()
                with ExitStack() as pst:
                    cur.append(pst)
                    usage.append(0)
                    fn(*a, **k)
                    S.barrier()
                    S.flush()
                    cur.pop()
                    _DBG.setdefault("phase_usage", []).append((fn.__name__, sum(usage)))
                    usage.pop()
            return wrapped

        class Ring:
            def __init__(self, name, n, shape, dt, ps=False):
                self.t = [(psum if ps else sb)(f"{name}{i}", shape, dt) for i in range(n)]
                self.k = [f"{name}{i}" for i in range(n)]
                self.i = 0

            def next(self):
                j = self.i % len(self.t)
                self.i += 1
                return self.t[j], self.k[j]

        castr = Ring("caststg", 3, [128, 1024], F32)

        def load_cast(dst, src, wkey):
            sh = tuple(dst.shape)
            if len(sh) == 2:
                stg, stgk = castr.next()
                S.dma("sp", stg[:, 0:sh[1]], src, writes=[stgk])
                S.op("pool", lambda e, stg=stg: e.tensor_copy(dst, stg[:, 0:sh[1]]), reads=[stgk], writes=[wkey])
                return
            A, B = sh[1], sh[2]
            step = max(1, 1024 // B)
            for a0 in range(0, A, step):
                a1 = min(A, a0 + step)
                stg, stgk = castr.next()
                sv = stg[:, 0:(a1 - a0) * B].rearrange("p (a b) -> p a b", b=B)
                S.dma("sp", sv, src[:, a0:a1, :], writes=[stgk])
                S.op("pool", lambda e, sv=sv, a0=a0, a1=a1: e.tensor_copy(dst[:, a0:a1, :], sv), reads=[stgk], writes=[wkey])

        def bcast_row(th, off, n):
            return bass.AP(th, off, [[0, 128], [1, n]])

        ident = sb("ident", [128, 128], BF16)
        identf = sb("identf", [128, 128], F32)
        onesf = sb("onesf", [128, 128], F32)
        ones1 = sb("ones1", [128, 128], F32)
        onesb = sb("onesb", [128, 128], BF16)
        epsb = sb("epsb", [128, 1], F32)
        flg = sb("flg", [128, 4], F32)
        maskf = sb("maskf", [128, 1056], F32)
        maskb = sb("maskb", [128, 256], BF16)
        maskhB = sb("maskhB", [128, 256], BF16)
        maskhC = sb("maskhC", [128, 256], BF16)
        smask = sb("smask", [128, 800], F32)

        S.op("pool", lambda e: e.memset(identf[:], 1.0), writes=["identf"])
        S.op("pool", lambda e: e.affine_select(identf[:], identf[:], pattern=[[-1, 128]], compare_op=ALU.is_equal,
                                               fill=0.0, base=0, channel_multiplier=1), reads=["identf"], writes=["identf"])
        S.op("dve", lambda e: e.tensor_copy(ident[:], identf[:]), reads=["identf"], writes=["ident"])
        S.op("pool", lambda e: e.memset(onesf[:], 1.0 / 512), writes=["onesf"])
        S.op("pool", lambda e: e.memset(ones1[:], 1.0), writes=["ones1"])
        S.op("pool", lambda e: e.memset(onesb[:], 1.0), writes=["onesb"])
        S.op("pool", lambda e: e.memset(epsb[:], EPS), writes=["epsb"])
        S.dma("sp", flg[:], flags.ap(), writes=["flg"])
        S.dma("sp", maskf[:], maskin.ap(), writes=["maskf"])
        S.op("dve", lambda e: e.tensor_copy(maskb[:], maskf[:, 0:256]), reads=["maskf"], writes=["maskb"])
        S.op("dve", lambda e: e.tensor_scalar(maskhB[:], maskf[:, 0:256], flg[:, 0:1], None, ALU.add),
             reads=["maskf", "flg"], writes=["maskhB"])
        S.op("dve", lambda e: e.tensor_scalar(maskhC[:], maskf[:, 0:256], flg[:, 1:2], None, ALU.add),
             reads=["maskf", "flg"], writes=["maskhC"])
        S.op("dve", lambda e: e.tensor_copy(smask[:], maskf[:, 256:1056]), reads=["maskf"], writes=["smask"])

        pb = [psum(f"pb{i}", [128, 512], F32) for i in range(7)]
        pT = psum("pT", [128, 1024], BF16)
        PB = [f"pb{i}" for i in range(7)]

        MOD = [None, None]
        MODK = ["MODP", "MODS"]
        modcur = {}
        zt = sb("zt", [128, 8, 64], BF16)
        S.op("pool", lambda e: e.memset(zt[:], 0.0), writes=["zt"])
        S.dma("sp", CAT.ap()[:, NPT * 128 + 64:NPT * 128 + 128].rearrange("(c p) t -> p c t", p=128), zt[:], reads=["zt"])

        xr = Ring("xr", 2, [128, D], F32)
        tmpr = Ring("tmpr", 2, [128, D], F32)
        hbr = Ring("hbr", 2, [128, D], BF16)
        str_ = Ring("st", 4, [128, 8], F32)
        junk = sb("junk", [128, D], BF16)

        def rms_stats(src_aps, skeys, nfeat):
            stt, sk = str_.next()
            for i, (a, k) in enumerate(zip(src_aps, skeys)):
                S.op("act", lambda e, a=a, i=i: e.activation(junk[:, 0:a.shape[-1]], a, AF.Square, accum_out=stt[:, i:i + 1]),
                     reads=[k], writes=["junk", sk])
            if len(src_aps) == 2:
                S.op("dve", lambda e: e.tensor_tensor(stt[:, 0:1], stt[:, 0:1], stt[:, 1:2], ALU.add), reads=[sk], writes=[sk])
            S.op("act", lambda e: e.activation(stt[:, 2:3], stt[:, 0:1], AF.Sqrt, bias=epsb[:, 0:1], scale=1.0 / nfeat),
                 reads=[sk, "epsb"], writes=[sk])
            S.op("dve", lambda e: e.reciprocal(stt[:, 3:4], stt[:, 2:3]), reads=[sk], writes=[sk])
            return stt, sk

        def transpose8(src, skey, dst_ap, dkey, eng="act"):
            for kc in range(8):
                S.op("pe", lambda e, kc=kc: e.transpose(pT[:, kc * 128:(kc + 1) * 128], src[:, kc * 128:(kc + 1) * 128], ident[:]),
                     reads=[skey, "ident"], writes=["pT"])
            pv = pT[:].rearrange("p (c t) -> p c t", c=8)
            if eng == "act":
                S.op("act", lambda e: e.copy(dst_ap, pv), reads=["pT"], writes=[dkey])
            else:
                S.op("dve", lambda e: e.tensor_copy(dst_ap, pv), reads=["pT"], writes=[dkey])

        @phase
        def phase_mod(l):
            MOD[0] = sb("MODP%d" % l, [128, 6 * D], F32)
            MOD[1] = sb("MODS%d" % l, [128, 6 * D], F32)
            wmr = Ring("wm%d_" % l, 3, [128, 8, 512], BF16)
            bmr = Ring("bm%d_" % l, 2, [128, 512], F32)
            gt_ = sb("gtmp%d" % l, [128, D], F32)
            cx = sb("cx%d" % l, [128, D], F32)
            cxb = sb("cxb%d" % l, [128, D], BF16)
            cT = [sb("cT%d_%d" % (l, s), [128, 8, 128], BF16) for s in range(2)]
            for s in range(2):
                S.dma("sp", cx[:], cexp.ap()[s], writes=["cx"])
                S.op("act", lambda e: e.activation(cxb[:], cx[:], AF.Silu), reads=["cx"], writes=["cxb"])
                transpose8(cxb, "cxb", cT[s][:], "cT%d" % s)
            for nb in range(12):
                wt, wk = wmr.next()
                load_cast(wt[:], w_mod.ap()[l][:, nb * 512:(nb + 1) * 512].rearrange("(kc p) n -> p kc n", p=128), wk)
                bt, bk = bmr.next()
                S.dma("sp", bt[:], bcast_row(b_mod, l * 6 * D + nb * 512, 512), writes=[bk])
                for s in range(2):
                    pp = pb[s]
                    for kc in range(8):
                        S.op("pe", lambda e, kc=kc, s=s, pp=pp, wt=wt: e.matmul(pp[:], cT[s][:, kc, :], wt[:, kc, :], start=(kc == 0), stop=(kc == 7)),
                             reads=["cT%d" % s, wk], writes=[PB[s]])
                    S.op("dve", lambda e, s=s, pp=pp, bt=bt, nb=nb: e.tensor_tensor(MOD[s][:, nb * 512:(nb + 1) * 512], pp[:], bt[:], ALU.add),
                         reads=[PB[s], bk], writes=[MODK[s]])
            for (slot, gvec, kind) in ((1, g_pre_mix, "A"), (2, g_post_mix, "G"), (4, g_pre_ffn, "A"), (5, g_post_ffn, "G")):
                S.dma("sp", gt_[:], bcast_row(gvec, l * D, D), writes=["gtmp"])
                for s in range(2):
                    sl = MOD[s][:, slot * D:(slot + 1) * D]
                    if kind == "A":
                        S.op("dve", lambda e, sl=sl: e.scalar_tensor_tensor(sl, sl, 1.0, gt_[:], ALU.add, ALU.mult),
                             reads=["gtmp", MODK[s]], writes=[MODK[s]])
                    else:
                        S.op("dve", lambda e, sl=sl: e.tensor_tensor(sl, sl, gt_[:], ALU.mult),
                             reads=["gtmp", MODK[s]], writes=[MODK[s]])
            for s in range(2):
                S.dma("sp", MODD.ap()[s].rearrange("k p d -> p k d"), MOD[s][:].rearrange("p (k d) -> p k d", k=6), reads=[MODK[s]], writes=[("MODD", s)])

        def load_mod(tag, slots, sets=(0, 1)):
            modcur.clear()
            for s in sets:
                t = sb("mc%s_%d" % (tag, s), [128, len(slots), D], F32)
                for i, slot in enumerate(slots):
                    S.dma("sp", t[:, i, :], MODD.ap()[s, slot], writes=["mc%d" % s])
                    modcur[(s, slot)] = (t[:, i, :], "mc%d" % s)

        def modsl(ti, slot):
            s = 1 if ti == NPT else 0
            return modcur[(s, slot)]

        def norm_mod_T(xt, xk, ti, aslot, bslot, dst_ap, dkey, fp32_out=None):
            stt, sk = rms_stats([xt[:]], [xk], D)
            A, ak = modsl(ti, aslot)
            B, bk = modsl(ti, bslot)
            tt, tk = tmpr.next()
            S.op("dve", lambda e: e.scalar_tensor_tensor(tt[:], xt[:], stt[:, 3:4], A, ALU.mult, ALU.mult),
                 reads=[xk, sk, ak], writes=[tk])
            hb, hk = hbr.next()
            if fp32_out is not None:
                fo, fk = fp32_out
                S.op("pool", lambda e: e.tensor_tensor(fo[:], tt[:], B, ALU.add), reads=[tk, bk], writes=[fk])
                S.op("pool", lambda e: e.tensor_copy(hb[:], fo[:]), reads=[fk], writes=[hk])
            else:
                S.op("pool", lambda e: e.tensor_tensor(hb[:], tt[:], B, ALU.add), reads=[tk, bk], writes=[hk])
            transpose8(hb, hk, dst_ap, dkey)

        @phase
        def phase_premix(l, tiles, XIN):
            load_mod("pm%d" % l, (0, 1))
            wres = sb("wresA%d" % l, [128, 8, 2560], BF16)
            for i in range(5):
                load_cast(wres[:, :, i * 512:(i + 1) * 512],
                          w_in.ap()[l][:, i * 512:(i + 1) * 512].rearrange("(kc p) n -> p kc n", p=128), "wres")
            hTr = Ring("hT%d_" % l, 2, [128, 8, 512], BF16)
            qkTr = Ring("qkT%d_" % l, 2, [128, 8, 512], BF16)
            qkr = Ring("qk%d_" % l, 2, [128, D], F32)
            vfr = Ring("vf%d_" % l, 2, [128, 512], F32)
            vbr = Ring("vb%d_" % l, 2, [128, 512], BF16)
            rpr = Ring("rp%d_" % l, 2, [128, 4, 128], F32)
            rtr = Ring("rt%d_" % l, 2, [128, 4, 128], F32)
            sgr = Ring("sg%d_" % l, 2, [128, 512], F32)
            ubr = Ring("ub%d_" % l, 2, [128, 4, 512], BF16)
            uf = sb("uf%d" % l, [128, 4, 128], F32)
            uo = sb("uo%d" % l, [128, 512], F32)
            blocks = []
            i = 0
            while i < len(tiles):
                if tiles[i] == NPT:
                    blocks.append([NPT]); i += 1
                else:
                    blocks.append(tiles[i:i + 4]); i += 4
            for blk in blocks:
                nt = len(blk)
                W = nt * 128
                t0 = blk[0] * 128
                hT, hTk = hTr.next()
                qkT, qkTk = qkTr.next()
                for j, ti in enumerate(blk):
                    xt, xk = xr.next()
                    S.dma("sp", xt[:], XIN.ap()[ti * 128:(ti + 1) * 128, :], reads=[("X", ti)], writes=[xk])
                    norm_mod_T(xt, xk, ti, 1, 0, hT[:, :, j * 128:(j + 1) * 128], hTk)
                    for n3 in range(3):
                        for kc in range(8):
                            S.op("pe", lambda e, n3=n3, kc=kc, j=j, hT=hT: e.matmul(pb[n3][:], hT[:, kc, j * 128:(j + 1) * 128],
                                                                                   wres[:, kc, n3 * 512:(n3 + 1) * 512], start=(kc == 0), stop=(kc == 7)),
                                 reads=[hTk, "wres"], writes=[PB[n3]])
                    qk, qkk = qkr.next()
                    S.op("act", lambda e, qk=qk: e.copy(qk[:, 0:512], pb[0][:]), reads=[PB[0]], writes=[qkk])
                    S.op("act", lambda e, qk=qk: e.copy(qk[:, 512:1024], pb[1][:]), reads=[PB[1]], writes=[qkk])
                    vf, vfk = vfr.next()
                    vb, vbk = vbr.next()
                    S.op("dve", lambda e, vf=vf: e.tensor_copy(vf[:], pb[2][:]), reads=[PB[2]], writes=[vfk])
                    S.op("pool", lambda e, vf=vf, vb=vb: e.tensor_copy(vb[:], vf[:]), reads=[vfk], writes=[vbk])
                    S.dma("sp", VV.ap()[ti * 128:(ti + 1) * 128, :], vb[:], reads=[vbk], writes=[("V", ti)])
                    rp, rpk = rpr.next()
                    S.dma("sp", rp[:], rope.ap()[ti * 128:(ti + 1) * 128], writes=[rpk])
                    rt, rtk = rtr.next()
                    qv = qk[:].rearrange("p (h d) -> p h d", d=64)
                    x1 = qv[:, :, 0:8]
                    x2 = qv[:, :, 8:16]
                    cosv = rp[:, 0, :].rearrange("p (h d) -> p h d", d=8)
                    sinv = rp[:, 1, :].rearrange("p (h d) -> p h d", d=8)
                    tv = [rt[:, i_, :].rearrange("p (h d) -> p h d", d=8) for i_ in range(4)]
                    S.op("dve", lambda e, x1=x1, cosv=cosv, tv=tv: e.tensor_tensor(tv[0], x1, cosv, ALU.mult), reads=[qkk, rpk], writes=[rtk])
                    S.op("dve", lambda e, x2=x2, sinv=sinv, tv=tv: e.tensor_tensor(tv[1], x2, sinv, ALU.mult), reads=[qkk, rpk], writes=[rtk])
                    S.op("dve", lambda e, x2=x2, cosv=cosv, tv=tv: e.tensor_tensor(tv[2], x2, cosv, ALU.mult), reads=[qkk, rpk], writes=[rtk])
                    S.op("dve", lambda e, x1=x1, sinv=sinv, tv=tv: e.tensor_tensor(tv[3], x1, sinv, ALU.mult), reads=[qkk, rpk], writes=[rtk])
                    S.op("dve", lambda e, x1=x1, tv=tv: e.tensor_tensor(x1, tv[0], tv[1], ALU.subtract), reads=[rtk], writes=[qkk])
                    S.op("dve", lambda e, x2=x2, tv=tv: e.tensor_tensor(x2, tv[2], tv[3], ALU.add), reads=[rtk], writes=[qkk])
                    if ti >= 32:
                        orow = (ti - 32) * 128
                        S.dma("sp", o_k.ap()[l][orow:orow + 128, :], qk[:, 512:1024], reads=[qkk])
                        S.dma("sp", o_v.ap()[l][orow:orow + 128, :], vf[:], reads=[vfk])
                    hb, hk = hbr.next()
                    S.op("pool", lambda e, hb=hb, qk=qk: e.tensor_copy(hb[:], qk[:]), reads=[qkk], writes=[hk])
                    transpose8(hb, hk, qkT[:, :, j * 128:(j + 1) * 128], qkTk, eng="dve")
                S.dma("sp", QT.ap()[:, t0:t0 + W].rearrange("(c p) t -> p c t", p=128), qkT[:, 0:4, 0:W], reads=[qkTk], writes=[("QT", t0)])
                S.dma("sp", KT.ap()[:, t0:t0 + W].rearrange("(c p) t -> p c t", p=128), qkT[:, 4:8, 0:W], reads=[qkTk], writes=[("KT", t0)])
                ub, ubk = ubr.next()
                for cc in range(4):
                    for which, pbi in ((0, 3), (1, 4)):
                        col = 1536 + which * 512 + cc * 128
                        for kc in range(8):
                            S.op("pe", lambda e, kc=kc, col=col, pbi=pbi, hT=hT, W=W: e.matmul(pb[pbi][:, 0:W], wres[:, kc, col:col + 128], hT[:, kc, 0:W],
                                                                                          start=(kc == 0), stop=(kc == 7)),
                                 reads=[hTk, "wres"], writes=[PB[pbi]])
                    sg, sgk = sgr.next()
                    S.op("act", lambda e, sg=sg, W=W: e.activation(sg[:, 0:W], pb[4][:, 0:W], AF.Sigmoid), reads=[PB[4]], writes=[sgk])
                    S.op("dve", lambda e, sg=sg, ub=ub, cc=cc, W=W: e.tensor_tensor(ub[:, cc, 0:W], pb[3][:, 0:W], sg[:, 0:W], ALU.mult),
                         reads=[PB[3], sgk], writes=[ubk])
                    if blk[-1] == 47 or blk[0] == NPT:
                        c0 = W - 128
                        S.op("dve", lambda e, sg=sg, cc=cc, c0=c0: e.tensor_tensor(uf[:, cc, :], pb[3][:, c0:c0 + 128], sg[:, c0:c0 + 128], ALU.mult),
                             reads=[PB[3], sgk], writes=["uf"])
                S.dma("sp", UT.ap()[:, t0:t0 + W].rearrange("(c p) t -> p c t", p=128), ub[:, :, 0:W], reads=[ubk], writes=[("UT", t0)])
                if blk[-1] == 47 or blk[0] == NPT:
                    for cc in range(4):
                        S.op("pe", lambda e, cc=cc: e.transpose(pb[5][:, cc * 128:(cc + 1) * 128], uf[:, cc, :], identf[:]),
                             reads=["uf", "identf"], writes=[PB[5]])
                    S.op("act", lambda e: e.copy(uo[:], pb[5][:]), reads=[PB[5]], writes=["uo"])
                    if blk[-1] == 47:
                        S.dma("sp", o_cp.ap()[l], uo[96:128, :], reads=["uo"])
                    else:
                        for s in range(16):
                            S.dma("sp", o_cs.ap()[l][s, 26:30, :], uo[4 * s:4 * s + 4, :], reads=["uo"])
                        S.dma("sp", o_cs.ap()[l][:, 0:26, :], sconv.ap()[l][:, 4:30, :])

        @phase
        def phase_attn(l, q0, maskh, mhk):
            k0 = q0 - 2048
            qhr = Ring("qh%d_%d_" % (l, q0), 2, [128, 2048], BF16)
            khr = Ring("kh%d_%d_" % (l, q0), 2, [128, 4096], BF16)
            vP = {d: sb("vP%d_%d_%d" % (l, q0, d), [128, 16 + d, 128], BF16) for d in (1, 4, 16)}
            Oacc = sb("Oacc%d_%d" % (l, q0), [128, 2048], F32)
            Dacc = sb("Dacc%d_%d" % (l, q0), [128, 2048], F32)
            att = sb("att%d_%d" % (l, q0), [128, 2048], BF16)
            ptr = Ring("pt%d_%d_" % (l, q0), 3, [128, 256], BF16)
            sc_i = [0]
            for h in range(8):
                p0 = 64 * (h % 2)
                pr = slice(p0, p0 + 64)
                qh, qhk = qhr.next()
                kh, khk = khr.next()
                S.dma("sp", qh[pr, :], QT.ap()[64 * h:64 * h + 64, q0:q0 + 2048], writes=[qhk])
                S.dma("sp", kh[pr, :], KT.ap()[64 * h:64 * h + 64, k0:k0 + 4096], writes=[khk])
                if h % 2 == 0:
                    pc = slice(64 * h, 64 * h + 128)
                    for d in (1, 4, 16):
                        nkb = 16 // d + 1
                        kb0 = 16 // d - 1
                        for r in range(d):
                            base = k0 + d * 128 * kb0 + r
                            src = bass.AP(VV, base * 512 + 64 * h, [[d * 512, 128], [128 * d * 512, nkb], [1, 128]])
                            S.dma("sp", vP[d][:, r * nkb:(r + 1) * nkb, :], src, writes=["vP%d" % d])
                first = {1: True, 4: True, 16: True}
                for d in (1, 4, 16):
                    nkb = 16 // d + 1
                    kb0 = 16 // d - 1
                    nqb = 16 // d
                    for r in range(d):
                        for kbi in range(nkb):
                            kb = kb0 + kbi
                            has_same = kbi >= 1
                            has_next = kbi < nkb - 1
                            if has_same and has_next:
                                msel = (0, 256)
                            elif has_next:
                                msel = (128, 256)
                            else:
                                msel = (0, 128)
                            N = msel[1] - msel[0]
                            jb_first = kb if has_same else kb + 1
                            qcol = d * 128 * jb_first + r - 2048
                            kcol = d * 128 * kb + r
                            spb = pb[sc_i[0] % 2]; spk = PB[sc_i[0] % 2]; sc_i[0] += 1
                            mk_ = maskh if kbi == 0 else maskb
                            mkk = mhk if kbi == 0 else "maskb"
                            S.op("pe", lambda e, spb=spb, kcol=kcol, qcol=qcol, N=N, d=d, pr=pr, kh=kh, qh=qh: e.matmul(
                                spb[:, 0:N], kh[pr, kcol:kcol + 127 * d + 1:d], qh[pr, qcol:qcol + (N - 1) * d + 1:d], start=True, stop=False),
                                reads=[khk, qhk], writes=[spk])
                            S.op("pe", lambda e, spb=spb, mk_=mk_, msel=msel, N=N: e.matmul(
                                spb[:, 0:N], ident[:], mk_[:, msel[0]:msel[1]], start=False, stop=True),
                                reads=["ident", mkk], writes=[spk])
                            pt, ptk = ptr.next()
                            S.op("act", lambda e, pt=pt, spb=spb, N=N: e.activation(pt[:, 0:N], spb[:, 0:N], AF.Exp, scale=0.125),
                                 reads=[spk], writes=[ptk])
                            vt = vP[d][:, r * nkb + kbi, :]
                            off = 0
                            for which in ("same", "next"):
                                if which == "same" and not has_same:
                                    continue
                                if which == "next" and not has_next:
                                    continue
                                jb = kb if which == "same" else kb + 1
                                qi = jb - 16 // d
                                gq = r * nqb + qi
                                bank = 2 + (gq // 4) % 2
                                dbank = 4 + (gq // 4) % 2
                                cs = (gq % 4) * 128
                                S.op("pe", lambda e, bank=bank, cs=cs, vt=vt, pt=pt, off=off, which=which: e.matmul(
                                    pb[bank][:, cs:cs + 128], vt, pt[:, off:off + 128], start=(which == "next"), stop=(which == "same")),
                                    reads=["vP%d" % d, ptk], writes=[PB[bank]])
                                S.op("pe", lambda e, dbank=dbank, cs=cs, pt=pt, off=off, which=which: e.matmul(
                                    pb[dbank][:, cs:cs + 128], onesb[:], pt[:, off:off + 128], start=(which == "next"), stop=(which == "same")),
                                    reads=["onesb", ptk], writes=[PB[dbank]])
                                off += 128
                                if which == "same" and gq % 4 == 3:
                                    g4 = gq // 4
                                    for (acc, bk_, akey) in ((Oacc, bank, "Oacc"), (Dacc, dbank, "Dacc")):
                                        if d == 1:
                                            dst = [(acc[pr, g4 * 512:(g4 + 1) * 512], pb[bk_][pr, :])]
                                        elif d == 4:
                                            dst = [(acc[pr, r:2048:4], pb[bk_][pr, :])]
                                        else:
                                            dst = [(acc[pr, (4 * g4 + rr):2048:16], pb[bk_][pr, rr * 128:(rr + 1) * 128]) for rr in range(4)]
                                        for (da, sa) in dst:
                                            if d == 1:
                                                S.op("dve", lambda e, da=da, sa=sa: e.tensor_copy(da, sa), reads=[PB[bk_]], writes=[akey])
                                            else:
                                                S.op("dve", lambda e, da=da, sa=sa: e.tensor_tensor(da, da, sa, ALU.add), reads=[PB[bk_], akey], writes=[akey])
                S.op("dve", lambda e, pr=pr: e.reciprocal(Dacc[pr, :], Dacc[pr, :]), reads=["Dacc"], writes=["Dacc"])
                S.op("dve", lambda e, pr=pr: e.tensor_tensor(att[pr, :], Oacc[pr, :], Dacc[pr, :], ALU.mult), reads=["Oacc", "Dacc"], writes=["att"])
                S.dma("sp", CAT.ap()[64 * h:64 * h + 64, q0:q0 + 2048], att[pr, :], reads=["att"], writes=[("CATa", h, q0)])

        def conv_consts(l):
            cw = sb("cw%d" % l, [128, 4, 31], F32)
            cvec = sb("cvec%d" % l, [128, 3, 4], F32)
            dg = sb("dg%d" % l, [128, 4, 31, 128], BF16)
            S.dma("sp", cw[:], conv_wT.ap()[l].rearrange("(c p) j -> p c j", p=128), writes=["cw"])
            for i, vec in enumerate((conv_b, conv_ln_g, conv_ln_b)):
                S.dma("sp", cvec[:, i, :], bass.AP(vec, l * 512, [[1, 128], [128, 4]]), writes=["cvec"], allow_slow_non_contiguous=True)
            for cc in range(4):
                for j in range(31):
                    S.op("dve", lambda e, cc=cc, j=j: e.tensor_scalar(dg[:, cc, j, :], identf[:], cw[:, cc, j:j + 1], None, ALU.mult),
                         reads=["identf", "cw"], writes=["dg"])
            return cvec, dg

        def conv_block(l, cvec, dg, rhs_fn, W, rkeys, out_cols, rings, oview=lambda a: a):
            cvr, sqr, mnr, cbr, t1r = rings
            cvf, cvk = cvr.next()
            sq, sqk = sqr.next()
            for cc in range(4):
                pc = pb[cc % 2]; pck = PB[cc % 2]
                for j in range(31):
                    rhs = rhs_fn(cc, j)
                    S.op("pe", lambda e, pc=pc, cc=cc, j=j, rhs=rhs: e.matmul(oview(pc[:, 0:W]), dg[:, cc, j, :], rhs, start=(j == 0), stop=(j == 30)),
                         reads=["dg"] + rkeys, writes=[pck])
                S.op("act", lambda e, pc=pc, cc=cc, cvf=cvf: e.activation(cvf[:, cc, 0:W], pc[:, 0:W], AF.Identity, bias=cvec[:, 0, cc:cc + 1]),
                     reads=[pck, "cvec"], writes=[cvk])
                S.op("act", lambda e, cc=cc, cvf=cvf, sq=sq: e.activation(sq[:, cc, 0:W], cvf[:, cc, 0:W], AF.Square), reads=[cvk], writes=[sqk])
            for cc in range(4):
                S.op("pe", lambda e, cc=cc, cvf=cvf: e.matmul(pb[2][:, 0:W], onesf[:], cvf[:, cc, 0:W], start=(cc == 0), stop=(cc == 3)),
                     reads=["onesf", cvk], writes=[PB[2]])
            for cc in range(4):
                S.op("pe", lambda e, cc=cc, sq=sq: e.matmul(pb[3][:, 0:W], onesf[:], sq[:, cc, 0:W], start=(cc == 0), stop=(cc == 3)),
                     reads=["onesf", sqk], writes=[PB[3]])
            mn, mnk = mnr.next()
            S.op("act", lambda e, mn=mn: e.copy(mn[:, 0, 0:W], pb[2][:, 0:W]), reads=[PB[2]], writes=[mnk])
            S.op("pool", lambda e, mn=mn: e.tensor_tensor(mn[:, 1, 0:W], mn[:, 0, 0:W], mn[:, 0, 0:W], ALU.mult), reads=[mnk], writes=[mnk])
            S.op("dve", lambda e, mn=mn: e.tensor_tensor(mn[:, 1, 0:W], pb[3][:, 0:W], mn[:, 1, 0:W], ALU.subtract), reads=[PB[3], mnk], writes=[mnk])
            S.op("dve", lambda e, mn=mn: e.tensor_scalar(mn[:, 1, 0:W], mn[:, 1, 0:W], 0.0, None, ALU.max), reads=[mnk], writes=[mnk])
            S.op("act", lambda e, mn=mn: e.activation(mn[:, 2, 0:W], mn[:, 1, 0:W], AF.Sqrt, bias=epsb[:, 0:1]), reads=[mnk, "epsb"], writes=[mnk])
            S.op("dve", lambda e, mn=mn: e.reciprocal(mn[:, 2, 0:W], mn[:, 2, 0:W]), reads=[mnk], writes=[mnk])
            cb, cbk = cbr.next()
            for cc in range(4):
                t1, t1k = t1r.next()
                S.op("dve", lambda e, cc=cc, t1=t1, cvf=cvf, mn=mn: e.tensor_tensor(t1[:, 0:W], cvf[:, cc, 0:W], mn[:, 0, 0:W], ALU.subtract),
                     reads=[cvk, mnk], writes=[t1k])
                S.op("pool", lambda e, t1=t1, mn=mn: e.tensor_tensor(t1[:, 0:W], t1[:, 0:W], mn[:, 2, 0:W], ALU.mult), reads=[t1k, mnk], writes=[t1k])
                S.op("act", lambda e, cc=cc, t1=t1, cb=cb: e.activation(cb[:, cc, 0:W], t1[:, 0:W], AF.Silu, bias=cvec[:, 2, cc:cc + 1], scale=cvec[:, 1, cc:cc + 1]),
                     reads=[t1k, "cvec"], writes=[cbk])
            S.dma("sp", CAT.ap()[512:1024, out_cols:out_cols + W].rearrange("(c p) t -> p c t", p=128), cb[:, :, 0:W], reads=[cbk],
                  writes=[("CATc", out_cols)])

        @phase
        def phase_conv(l, starts):
            cvec, dg = conv_consts(l)
            rings = (Ring("cvf%d_" % l, 2, [128, 4, 512], F32), Ring("csq%d_" % l, 2, [128, 4, 512], F32),
                     Ring("cmn%d_" % l, 2, [128, 3, 512], F32), Ring("ccb%d_" % l, 2, [128, 4, 512], BF16),
                     Ring("ct1%d_" % l, 2, [128, 512], F32))
            ubr = Ring("cub%d_" % l, 2, [128, 4, 544], BF16)
            for (tok0, fcol) in starts:
                ub, ubk = ubr.next()
                S.dma("sp", ub[:], UT.ap()[:, tok0 - 32:tok0 + 512].rearrange("(c p) t -> p c t", p=128), writes=[ubk])
                if fcol is not None:
                    S.op("dve", lambda e, ub=ub, fcol=fcol: e.tensor_scalar(ub[:, :, 0:32], ub[:, :, 0:32], flg[:, fcol:fcol + 1], None, ALU.mult),
                         reads=[ubk, "flg"], writes=[ubk])
                conv_block(l, cvec, dg, lambda cc, j, ub=ub: ub[:, cc, 2 + j:2 + j + 512], 512, [ubk], tok0, rings)
            usf = sb("usf%d" % l, [128, 4, 16, 34], BF16)
            stt_ = Ring("sst%d_" % l, 2, [120, 512], F32)
            un = sb("sun%d" % l, [128, 4, 128], BF16)
            S.dma("sp", un[:], UT.ap()[:, NPT * 128:NPT * 128 + 128].rearrange("(c p) t -> p c t", p=128), writes=["sun"])
            for g in range(4):
                stile, stk = stt_.next()
                S.dma("sp", stile[:], sconv.ap()[l][4 * g:4 * g + 4].rearrange("s j c -> (s j) c"), writes=[stk])
                for cc in range(4):
                    S.op("pe", lambda e, cc=cc, stile=stile: e.transpose(pb[4][:, cc * 120:(cc + 1) * 120], stile[:, cc * 128:(cc + 1) * 128], identf[0:120, 0:120]),
                         reads=[stk, "identf"], writes=[PB[4]])
                for cc in range(4):
                    S.op("act", lambda e, cc=cc, g=g: e.copy(usf[:, cc, 4 * g:4 * g + 4, 0:30],
                                                             pb[4][:, cc * 120:(cc + 1) * 120].rearrange("p (s j) -> p s j", j=30)),
                         reads=[PB[4]], writes=["usf"])
            for cc in range(4):
                S.op("dve", lambda e, cc=cc: e.tensor_copy(usf[:, cc, :, 30:34], un[:, cc, 0:64].rearrange("p (s t) -> p s t", t=4)),
                     reads=["sun"], writes=["usf"])
            conv_block(l, cvec, dg, lambda cc, j: usf[:, cc, :, j:j + 4], 64, ["usf"], NPT * 128, rings,
                       oview=lambda a: a.rearrange("p (s t) -> p s t", t=4))

        def router(l, ti, h2f, h2k):
            wr = router_w
            hTf = sb_router["hTf"]
            for kc in range(8):
                S.op("pe", lambda e, kc=kc: e.transpose(pb[5][:, (kc % 4) * 128:(kc % 4 + 1) * 128], h2f[:, kc * 128:(kc + 1) * 128], identf[:]),
                     reads=[h2k, "identf"], writes=[PB[5]])
                if kc % 4 == 3:
                    S.op("act", lambda e, kc=kc: e.copy(hTf[:, (kc // 4) * 512:(kc // 4 + 1) * 512], pb[5][:]), reads=[PB[5]], writes=["hTf"])
            for kc in range(8):
                S.op("pe", lambda e, kc=kc: e.matmul(pb[6][:, 0:8], hTf[:, kc * 128:(kc + 1) * 128], wr[:, kc, :], start=(kc == 0), stop=(kc == 7)),
                     reads=["hTf", "wr"], writes=[PB[6]])
            g, gk = sb_router["ring"].next()
            lg = g[:, 0:8]; eq1 = g[:, 8:16]; l2 = g[:, 16:24]; eq2 = g[:, 24:32]; gate = g[:, 32:40]
            m1 = g[:, 40:41]; m2 = g[:, 41:42]; dm = g[:, 42:43]; ex = g[:, 43:44]; w1 = g[:, 44:45]; w2 = g[:, 45:46]
            S.op("dve", lambda e: e.tensor_copy(lg, pb[6][:, 0:8]), reads=[PB[6]], writes=[gk])
            S.op("dve", lambda e: e.tensor_reduce(m1, lg, AX.X, ALU.max), reads=[gk], writes=[gk])
            S.op("dve", lambda e: e.tensor_scalar(eq1, lg, m1, None, ALU.is_equal), reads=[gk], writes=[gk])
            S.op("dve", lambda e: e.scalar_tensor_tensor(l2, eq1, -1e30, lg, ALU.mult, ALU.add), reads=[gk], writes=[gk])
            S.op("dve", lambda e: e.tensor_reduce(m2, l2, AX.X, ALU.max), reads=[gk], writes=[gk])
            S.op("dve", lambda e: e.tensor_scalar(eq2, l2, m2, None, ALU.is_equal), reads=[gk], writes=[gk])
            S.op("dve", lambda e: e.tensor_tensor(dm, m2, m1, ALU.subtract), reads=[gk], writes=[gk])
            S.op("act", lambda e: e.activation(ex, dm, AF.Exp), reads=[gk], writes=[gk])
            S.op("dve", lambda e: e.tensor_scalar(w1, ex, 1.0, None, ALU.add), reads=[gk], writes=[gk])
            S.op("dve", lambda e: e.reciprocal(w1, w1), reads=[gk], writes=[gk])
            S.op("dve", lambda e: e.tensor_tensor(w2, ex, w1, ALU.mult), reads=[gk], writes=[gk])
            S.op("dve", lambda e: e.tensor_scalar(gate, eq1, w1, None, ALU.mult), reads=[gk], writes=[gk])
            S.op("dve", lambda e: e.scalar_tensor_tensor(gate, eq2, w2, gate, ALU.mult, ALU.add), reads=[gk], writes=[gk])
            S.dma("sp", GATES.ap()[ti * 128:(ti + 1) * 128, :], gate, reads=[gk], writes=[("GATES", ti)])

        router_w = None
        sb_router = {}

        @phase
        def phase_postmix(l, tiles, XIN):
            nonlocal router_w
            load_mod("po%d" % l, (2, 3, 4))
            wres = sb("wresB%d" % l, [128, 8, 1024], BF16)
            for i in range(2):
                load_cast(wres[:, :, i * 512:(i + 1) * 512],
                          w_out.ap()[l][:, i * 512:(i + 1) * 512].rearrange("(kc p) n -> p kc n", p=128), "wres")
            catr = Ring("cat%d_" % l, 2, [128, 8, 512], BF16)
            h2Tr = Ring("h2T%d_" % l, 2, [128, 8, 512], BF16)
            x1r = Ring("x1%d_" % l, 2, [128, D], F32)
            h2fr = Ring("h2f%d_" % l, 2, [128, D], F32)
            if l == 1:
                router_w = sb("wr", [128, 8, 8], F32)
                S.dma("sp", router_w[:], moe_w_router.ap()[0].rearrange("(kc p) n -> p kc n", p=128), writes=["wr"])
                sb_router["hTf"] = sb("hTf", [128, 1024], F32)
                sb_router["ring"] = Ring("rg", 2, [128, 64], F32)
            blocks = []
            i = 0
            while i < len(tiles):
                if tiles[i] == NPT:
                    blocks.append([NPT]); i += 1
                else:
                    blocks.append(tiles[i:i + 4]); i += 4
            for blk in blocks:
                W = len(blk) * 128
                t0 = blk[0] * 128
                cat, catk = catr.next()
                S.dma("sp", cat[:, :, 0:W], CAT.ap()[:, t0:t0 + W].rearrange("(c p) t -> p c t", p=128), writes=[catk])
                h2T, h2Tk = h2Tr.next()
                for j, ti in enumerate(blk):
                    for half in range(2):
                        for kc in range(8):
                            S.op("pe", lambda e, half=half, kc=kc, j=j, cat=cat: e.matmul(pb[half][:], cat[:, kc, j * 128:(j + 1) * 128],
                                                                                       wres[:, kc, half * 512:(half + 1) * 512], start=(kc == 0), stop=(kc == 7)),
                                 reads=[catk, "wres"], writes=[PB[half]])
                    xt, xk = xr.next()
                    S.dma("sp", xt[:], XIN.ap()[ti * 128:(ti + 1) * 128, :], writes=[xk])
                    stt, sk = rms_stats([pb[0][:], pb[1][:]], [PB[0], PB[1]], D)
                    G1, g1k = modsl(ti, 2)
                    tt, tk = tmpr.next()
                    for half in range(2):
                        S.op("dve", lambda e, half=half, tt=tt, stt=stt, G1=G1: e.scalar_tensor_tensor(
                            tt[:, half * 512:(half + 1) * 512], pb[half][:], stt[:, 3:4], G1[:, half * 512:(half + 1) * 512], ALU.mult, ALU.mult),
                            reads=[PB[half], sk, g1k], writes=[tk])
                    x1, x1k = x1r.next()
                    S.op("pool", lambda e, x1=x1, tt=tt, xt=xt: e.tensor_tensor(x1[:], tt[:], xt[:], ALU.add), reads=[tk, xk], writes=[x1k])
                    S.dma("sp", XA.ap()[ti * 128:(ti + 1) * 128, :], x1[:], reads=[x1k], writes=[("XA", ti)])
                    if l == 1:
                        h2f, h2k = h2fr.next()
                        norm_mod_T(x1, x1k, ti, 4, 3, h2T[:, :, j * 128:(j + 1) * 128], h2Tk, fp32_out=(h2f, h2k))
                        router(l, ti, h2f, h2k)
                    else:
                        norm_mod_T(x1, x1k, ti, 4, 3, h2T[:, :, j * 128:(j + 1) * 128], h2Tk)
                S.dma("sp", H2T.ap()[:, t0:t0 + W].rearrange("(c p) t -> p c t", p=128), h2T[:, :, 0:W], reads=[h2Tk], writes=[("H2T", t0)])

        @phase
        def phase_ffn(l, gi, blocks, experts, XOUT, out_map):
            Tg = sum(w for _, w in blocks)
            FFD = int(os.environ.get("KDBG_FFN", "3"))
            nm = "f%d_%d" % (l, gi)
            load_mod(nm, (5,))
            H2 = sb(nm + "H2", [128, 8, Tg], BF16)
            facc = sb(nm + "fa", [128, 8, Tg], F32)
            moe = len(experts) > 1
            offs = []
            o = 0
            for (t0, W) in blocks:
                S.dma("sp", H2[:, :, o:o + W], H2T.ap()[:, t0:t0 + W].rearrange("(c p) t -> p c t", p=128), writes=[nm + "H2"])
                offs.append(o); o += W
            if moe:
                GBr = Ring(nm + "GB", 2, [128, Tg], BF16)
                gts = sb(nm + "gts", [128, Tg // 128, 8], F32)
                dgr = Ring(nm + "dgm", 2, [128, 128], F32)
                for bi, (t0, W) in enumerate(blocks):
                    for j in range(W // 128):
                        S.dma("sp", gts[:, offs[bi] // 128 + j, :], GATES.ap()[t0 + j * 128:t0 + (j + 1) * 128, :], writes=[nm + "gts"])
            wgr = Ring(nm + "wg", 2, [128, 8, 512], BF16)
            wur = Ring(nm + "wu", 2, [128, 8, 512], BF16)
            wdr = Ring(nm + "wd", 1, [128, 4, 1024], BF16)
            stgr = Ring(nm + "stg", 3, [128, 2, 512], F32)
            sgr = Ring(nm + "sg", 2, [128, 512], BF16)
            hidr = Ring(nm + "hid", 2, [128, 4, 512], BF16)
            first = True
            pgi = [0]
            for ex, (wg_ap, wu_ap, wd_ap) in enumerate(experts):
                if moe:
                    GB, gbk = GBr.next()
                    for jt in range(Tg // 128):
                        dgm, dgk = dgr.next()
                        S.op("dve", lambda e, dgm=dgm, jt=jt, ex=ex: e.tensor_scalar(dgm[:], identf[:], gts[:, jt, ex:ex + 1], None, ALU.mult),
                             reads=["identf", nm + "gts"], writes=[dgk])
                        S.op("pe", lambda e, dgm=dgm: e.matmul(pb[6][:, 0:128], ones1[:], dgm[:], start=True, stop=True),
                             reads=["ones1", dgk], writes=[PB[6]])
                        S.op("act", lambda e, GB=GB, jt=jt: e.copy(GB[:, jt * 128:(jt + 1) * 128], pb[6][:, 0:128]), reads=[PB[6]], writes=[gbk])
                for hg in range(7):
                    wg, wgk = wgr.next(); wu, wuk = wur.next(); wd, wdk = wdr.next()
                    for q4 in range(4):
                        for (wt_, wk_, src_) in ((wg, wgk, wg_ap), (wu, wuk, wu_ap)):
                            stg, stgk = stgr.next()
                            S.dma("sp", stg[:], src_[q4 * 256:(q4 + 1) * 256, hg * 512:(hg + 1) * 512].rearrange("(kc p) n -> p kc n", p=128), writes=[stgk])
                            S.op("pool", lambda e, wt_=wt_, stg=stg, q4=q4: e.tensor_copy(wt_[:, 2 * q4:2 * q4 + 2, :], stg[:]), reads=[stgk], writes=[wk_])
                    for q4 in range(4):
                        stg, stgk = stgr.next()
                        S.dma("sp", stg[:], wd_ap[hg * 512 + (q4 // 2) * 256:hg * 512 + (q4 // 2 + 1) * 256, (q4 % 2) * 512:(q4 % 2 + 1) * 512].rearrange("(kc p) n -> p kc n", p=128), writes=[stgk])
                        S.op("pool", lambda e, wd=wd, stg=stg, q4=q4: e.tensor_copy(wd[:, 2 * (q4 // 2):2 * (q4 // 2) + 2, (q4 % 2) * 512:(q4 % 2 + 1) * 512], stg[:]), reads=[stgk], writes=[wdk])
                    for bi, (t0, W) in enumerate(blocks):
                        o = offs[bi]
                        hid, hidk = hidr.next()
                        for mc in range(4):
                            pg = pb[0 + pgi[0] % 2]; pgk = PB[0 + pgi[0] % 2]
                            pu = pb[2 + pgi[0] % 2]; puk = PB[2 + pgi[0] % 2]
                            pgi[0] += 1
                            for kc in range(8):
                                S.op("pe", lambda e, pg=pg, wg=wg, kc=kc, mc=mc, o=o, W=W: e.matmul(pg[:, 0:W], wg[:, kc, mc * 128:(mc + 1) * 128], H2[:, kc, o:o + W],
                                                                                                  start=(kc == 0), stop=(kc == 7)),
                                     reads=[wgk, nm + "H2"], writes=[pgk])
                            for kc in range(8):
                                S.op("pe", lambda e, pu=pu, wu=wu, kc=kc, mc=mc, o=o, W=W: e.matmul(pu[:, 0:W], wu[:, kc, mc * 128:(mc + 1) * 128], H2[:, kc, o:o + W],
                                                                                                  start=(kc == 0), stop=(kc == 7)),
                                     reads=[wuk, nm + "H2"], writes=[puk])
                            sg, sgk = sgr.next()
                            S.op("act", lambda e, sg=sg, pg=pg, W=W: e.activation(sg[:, 0:W], pg[:, 0:W], AF.Silu), reads=[pgk], writes=[sgk])
                            if moe:
                                S.op("pool", lambda e, sg=sg, GB=GB, o=o, W=W: e.tensor_tensor(sg[:, 0:W], sg[:, 0:W], GB[:, o:o + W], ALU.mult),
                                     reads=[sgk, gbk], writes=[sgk])
                            S.op("dve", lambda e, hid=hid, mc=mc, pu=pu, sg=sg, W=W: e.tensor_tensor(hid[:, mc, 0:W], pu[:, 0:W], sg[:, 0:W], ALU.mult),
                                 reads=[puk, sgk], writes=[hidk])
                        for fc in range(8 if FFD >= 2 else 0):
                            pd = pb[4 + fc % 2]; pdk = PB[4 + fc % 2]
                            for mc in range(4):
                                S.op("pe", lambda e, pd=pd, wd=wd, mc=mc, fc=fc, hid=hid, W=W: e.matmul(pd[:, 0:W], wd[:, mc, fc * 128:(fc + 1) * 128], hid[:, mc, 0:W],
                                                                                                      start=(mc == 0), stop=(mc == 3)),
                                     reads=[wdk, hidk], writes=[pdk])
                            fk = (nm + "fa", bi, fc)
                            if first:
                                S.op("dve", lambda e, pd=pd, fc=fc, o=o, W=W: e.tensor_copy(facc[:, fc, o:o + W], pd[:, 0:W]), reads=[pdk], writes=[fk])
                            else:
                                S.op("dve", lambda e, pd=pd, fc=fc, o=o, W=W: e.tensor_tensor(facc[:, fc, o:o + W], facc[:, fc, o:o + W], pd[:, 0:W], ALU.add),
                                     reads=[pdk, fk], writes=[fk])
                    first = False
            if FFD < 3:
                return
            for bi, (t0, W) in enumerate(blocks):
                for j in range(W // 128):
                    ti = t0 // 128 + j
                    c0 = offs[bi] + j * 128
                    for fc in range(8):
                        S.op("pe", lambda e, fc=fc, c0=c0: e.transpose(pb[fc // 4][:, (fc % 4) * 128:(fc % 4 + 1) * 128], facc[:, fc, c0:c0 + 128], identf[:]),
                             reads=[(nm + "fa", bi, fc), "identf"], writes=[PB[fc // 4]])
                    xt, xk = xr.next()
                    S.dma("sp", xt[:], XA.ap()[ti * 128:(ti + 1) * 128, :], writes=[xk])
                    stt, sk = rms_stats([pb[0][:], pb[1][:]], [PB[0], PB[1]], D)
                    G2, g2k = modsl(ti, 5)
                    tt, tk = tmpr.next()
                    for half in range(2):
                        S.op("dve", lambda e, half=half, tt=tt, stt=stt, G2=G2: e.scalar_tensor_tensor(
                            tt[:, half * 512:(half + 1) * 512], pb[half][:], stt[:, 3:4], G2[:, half * 512:(half + 1) * 512], ALU.mult, ALU.mult),
                            reads=[PB[half], sk, g2k], writes=[tk])
                    x2, x2k = tt, tk
                    S.op("pool", lambda e, tt=tt, xt=xt: e.tensor_tensor(tt[:], tt[:], xt[:], ALU.add), reads=[tk, xk], writes=[tk])
                    dst = out_map(ti)
                    if dst is not None:
                        S.dma("sp", dst, x2[:], reads=[x2k])

        @phase
        def phase_sattn(l):
            nm = "sa%d" % l
            qT = sb(nm + "qT", [128, 4, 128], BF16)
            kT = sb(nm + "kT", [128, 4, 128], BF16)
            S.dma("sp", qT[:], QT.ap()[:, NPT * 128:NPT * 128 + 128].rearrange("(c p) t -> p c t", p=128), writes=[nm + "qT"])
            S.dma("sp", kT[:], KT.ap()[:, NPT * 128:NPT * 128 + 128].rearrange("(c p) t -> p c t", p=128), writes=[nm + "kT"])
            kfr = Ring(nm + "kf", 3, [128, 512], F32)
            kbr = Ring(nm + "kb", 3, [128, 512], BF16)
            kTr = Ring(nm + "kTt", 3, [128, 4, 128], BF16)
            vbr = Ring(nm + "vb", 18, [128, 512], BF16)
            vn = sb(nm + "vn", [128, 512], BF16)
            S.dma("sp", vn[:], VV.ap()[NPT * 128:NPT * 128 + 128, :], writes=[nm + "vn"])
            pnr = Ring(nm + "pn", 2, [128, 32], BF16)
            ptr = Ring(nm + "pt", 2, [128, 288], BF16)
            pf = sb(nm + "pf", [128, 320], F32)
            Osb = sb(nm + "O", [128, 32], F32)
            Dsb = sb(nm + "D", [128, 32], F32)
            attS = sb(nm + "att", [128, 4, 128], BF16)
            S.op("pool", lambda e: e.memset(attS[:], 0.0), writes=[nm + "att"])
            tl = [(1920, 1)] + [(1536 + r, 4) for r in range(4)] + [(r, 16) for r in range(4)]
            SA = int(os.environ.get("KDBG_SA", "99"))
            for s in range(16 if SA >= 99 else 1):
                vts = []
                for ti_, (row0, stride) in enumerate(tl):
                    kf, kfk = kfr.next()
                    S.dma("sp", kf[:], bass.AP(ck, ((l * 16 + s) * 2048 + row0) * 512, [[stride * 512, 128], [1, 512]]), writes=[kfk])
                    kb, kbk = kbr.next()
                    S.op("pool", lambda e, kb=kb, kf=kf: e.tensor_copy(kb[:], kf[:]), reads=[kfk], writes=[kbk])
                    vb, vbk = vbr.next()
                    if SA >= 2:
                        load_cast(vb[:], bass.AP(cv, ((l * 16 + s) * 2048 + row0) * 512, [[stride * 512, 128], [1, 512]]), vbk)
                    vts.append((vb, vbk))
                    if SA < 3:
                        continue
                    for c in range(4):
                        S.op("pe", lambda e, c=c, kb=kb: e.transpose(pT[:, c * 128:(c + 1) * 128], kb[:, c * 128:(c + 1) * 128], ident[:]),
                             reads=[kbk, "ident"], writes=["pT"])
                    kTt, kTk = kTr.next()
                    S.op("dve", lambda e, kTt=kTt: e.tensor_copy(kTt[:], pT[:, 0:512].rearrange("p (c t) -> p c t", c=4)), reads=["pT"], writes=[kTk])
                    for h in range(8):
                        p0 = 64 * (h % 2); pr = slice(p0, p0 + 64)
                        oc = h * 36 + ti_ * 4
                        S.op("pe", lambda e, h=h, pr=pr, oc=oc, kTt=kTt, s=s: e.matmul(pb[0][:, oc:oc + 4], kTt[pr, h // 2, :], qT[pr, h // 2, 4 * s:4 * s + 4], start=True, stop=True),
                             reads=[kTk, nm + "qT"], writes=[PB[0]])
                if SA < 4:
                    continue
                for h in range(8):
                    p0 = 64 * (h % 2); pr = slice(p0, p0 + 64)
                    S.op("pe", lambda e, h=h, pr=pr, s=s: e.matmul(pb[1][:, h * 4:h * 4 + 4], kT[pr, h // 2, :], qT[pr, h // 2, 4 * s:4 * s + 4], start=True, stop=True),
                         reads=[nm + "kT", nm + "qT"], writes=[PB[1]])
                if SA < 5:
                    continue
                pt, ptk = ptr.next()
                S.op("act", lambda e: e.activation(pf[:, 0:288], pb[0][:, 0:288], AF.Exp, scale=0.125), reads=[PB[0]], writes=[nm + "pf"])
                S.op("act", lambda e: e.activation(pf[:, 288:320], pb[1][:, 0:32], AF.Exp, scale=0.125), reads=[PB[1]], writes=[nm + "pf"])
                S.op("dve", lambda e, pt=pt: e.tensor_tensor(pt[:], pf[:, 0:288], smask[:, 0:288], ALU.mult), reads=[nm + "pf", "smask"], writes=[ptk])
                pn, pnk = pnr.next()
                S.op("dve", lambda e, pn=pn, s=s: e.tensor_tensor(pn[:], pf[:, 288:320], smask[:, 288 + 32 * s:320 + 32 * s], ALU.mult), reads=[nm + "pf", "smask"], writes=[pnk])
                if SA < 6:
                    continue
                vnn, vnk = vn, nm + "vn"
                for h in range(8):
                    hp = h // 2
                    oc = h * 4
                    for ti_ in range(9):
                        vb, vbk = vts[ti_]
                        pc = h * 36 + ti_ * 4
                        S.op("pe", lambda e, vb=vb, hp=hp, oc=oc, pc=pc, pt=pt, ti_=ti_: e.matmul(pb[2][:, oc:oc + 4], vb[:, hp * 128:(hp + 1) * 128], pt[:, pc:pc + 4], start=(ti_ == 0), stop=False),
                             reads=[vbk, ptk], writes=[PB[2]])
                        S.op("pe", lambda e, oc=oc, pc=pc, pt=pt, ti_=ti_: e.matmul(pb[3][:, oc:oc + 4], onesb[:], pt[:, pc:pc + 4], start=(ti_ == 0), stop=False),
                             reads=["onesb", ptk], writes=[PB[3]])
                    S.op("pe", lambda e, hp=hp, oc=oc, h=h, vnn=vnn, pn=pn: e.matmul(pb[2][:, oc:oc + 4], vnn[:, hp * 128:(hp + 1) * 128], pn[:, h * 4:h * 4 + 4], start=False, stop=True),
                         reads=[vnk, pnk], writes=[PB[2]])
                    S.op("pe", lambda e, oc=oc, h=h, pn=pn: e.matmul(pb[3][:, oc:oc + 4], onesb[:], pn[:, h * 4:h * 4 + 4], start=False, stop=True),
                         reads=["onesb", pnk], writes=[PB[3]])
                if SA < 7:
                    continue
                S.op("dve", lambda e: e.reciprocal(Dsb[:], pb[3][:, 0:32]), reads=[PB[3]], writes=[nm + "D"])
                S.op("dve", lambda e: e.tensor_tensor(Osb[:], pb[2][:, 0:32], Dsb[:], ALU.mult), reads=[PB[2], nm + "D"], writes=[nm + "O"])
                for h in range(8):
                    p0 = 64 * (h % 2); pr = slice(p0, p0 + 64)
                    S.op("pool", lambda e, h=h, pr=pr, s=s: e.tensor_copy(attS[pr, h // 2, 4 * s:4 * s + 4], Osb[pr, h * 4:h * 4 + 4]),
                         reads=[nm + "O"], writes=[nm + "att"])
            S.dma("sp", CAT.ap()[0:512, NPT * 128:NPT * 128 + 128].rearrange("(c p) t -> p c t", p=128), attS[:], reads=[nm + "att"], writes=[("CATa", "s")])

        Bt = list(range(16, 32)); Ct = list(range(32, 48)); At = list(range(0, 16))
        XIN0 = xin

        def ymap(ti):
            if ti >= 32:
                r = (ti - 32) * 128
                return o_y.ap()[r:r + 128, :]
            return None

        def xbmap(ti):
            return XB.ap()[ti * 128:(ti + 1) * 128, :]

        def blocks_of(tiles):
            out = []
            i = 0
            while i < len(tiles):
                if tiles[i] == NPT:
                    out.append((NPT * 128, 128)); i += 1
                else:
                    out.append((tiles[i] * 128, 512)); i += 4
            return out

        phase_mod(0)
        phase_premix(0, At + Bt + Ct + [NPT], xin)
        phase_attn(0, 2048, maskhB, "maskhB")
        phase_attn(0, 4096, maskhC, "maskhC")
        phase_sattn(0)
        phase_conv(0, [(2048 + 512 * i, 2 if i == 0 else None) for i in range(4)] + [(4096 + 512 * i, 3 if i == 0 else None) for i in range(4)])
        phase_postmix(0, Bt + Ct + [NPT], xin)
        dense = [(ffn_w_gate.ap()[0], ffn_w_up.ap()[0], ffn_w_down.ap()[0])]
        phase_ffn(0, 0, blocks_of(Bt[:8]), dense, XB, xbmap)
        phase_ffn(0, 1, blocks_of(Bt[8:]), dense, XB, xbmap)
        phase_ffn(0, 2, blocks_of(Ct[:8]), dense, XB, xbmap)
        last9 = [(40 * 128, 384), (43 * 128, 384), (46 * 128, 384)]
        phase_ffn(0, 3, last9, dense, XB, xbmap)
        phase_mod(1)
        phase_premix(1, Bt + Ct + [NPT], XB)
        phase_attn(1, 4096, maskhC, "maskhC")
        phase_sattn(1)
        phase_conv(1, [(4096 + 512 * i, 3 if i == 0 else None) for i in range(4)])
        phase_postmix(1, Ct + [NPT], XB)
        moe = [(moe_w_gate.ap()[0][e], moe_w_up.ap()[0][e], moe_w_down.ap()[0][e]) for e in range(8)]
        phase_ffn(1, 0, blocks_of(Ct[:8]), moe, None, ymap)
        phase_ffn(1, 1, last9, moe, None, ymap)
        S.final_wait("sp")
        S.flush()
        _DBG["cnt"] = dict(S.cnt)
        _DBG["umax"] = tuple(umax)
        _DBG["slots"] = {q: max(v) for q, v in S.slot_uses.items()}
    return nc


_NC_CACHE = {}
_DBG = {}


def _host_consts():
    m = np.arange(128)[:, None]
    a = np.arange(128)[None, :]
    lower = np.where(m <= a, 0.0, NEG)
    upper = np.where(m >= a, 0.0, NEG)
    mask = np.zeros((128, 1056), np.float32)
    mask[:, 0:128] = lower
    mask[:, 128:256] = upper
    sm = np.zeros((128, 288), np.float32)
    for h in range(8):
        for t in range(4):
            sm[:, h * 36 + t] = (np.arange(128) >= t).astype(np.float32)
            for pat in range(2):
                sm[:, h * 36 + 4 * (1 + 4 * pat + t) + t] = 1.0
    mask[:, 256:544] = sm
    nk = np.zeros((4, 4), np.float32)
    for tp in range(4):
        for t in range(4):
            nk[tp, t] = 1.0 if tp < t else (3.0 if tp == t else 0.0)
    for s_ in range(16):
        mask[4 * s_:4 * s_ + 4, 544 + 32 * s_:544 + 32 * s_ + 32] = np.tile(nk, (1, 8))
    return mask


def _rope_tables(pos):
    half = 8
    inv_freq = (np.float32(500000.0) ** (-np.arange(half, dtype=np.float32) * np.float32(2.0) / np.float32(16))).astype(np.float32)
    ang = pos.astype(np.float32)[:, None] * inv_freq[None, :]
    cos = np.cos(ang).astype(np.float32)
    sin = np.sin(ang).astype(np.float32)
    out = np.zeros((pos.shape[0], 4, 128), np.float32)
    out[:, 0, :] = np.tile(cos, (1, 16))
    out[:, 1, :] = np.tile(sin, (1, 16))
    return out


def kernel(x_prompt, x_sample, cache_k, cache_v, state_conv, c_prompt, c_sample,
           w_mod, b_mod, g_pre_mix, g_post_mix, g_pre_ffn, g_post_ffn, w_in,
           conv_w, conv_b, conv_ln_g, conv_ln_b, w_out,
           ffn_w_gate, ffn_w_up, ffn_w_down,
           moe_w_router, moe_w_gate, moe_w_up, moe_w_down):
    f = lambda a: np.ascontiguousarray(np.asarray(a, dtype=np.float32))
    x_prompt, x_sample, cache_k, cache_v, state_conv = map(f, (x_prompt, x_sample, cache_k, cache_v, state_conv))
    c_prompt, c_sample = f(c_prompt), f(c_sample)
    import os
    if os.environ.get("KDBG_ZS"):
        x_sample = x_sample * 0
    if "nc" not in _NC_CACHE:
        _NC_CACHE["nc"] = build_nc()
    nc = _NC_CACHE["nc"]
    mask = _host_consts()
    shared = {
        "w_mod": f(w_mod), "b_mod": f(b_mod), "g_pre_mix": f(g_pre_mix), "g_post_mix": f(g_post_mix),
        "g_pre_ffn": f(g_pre_ffn), "g_post_ffn": f(g_post_ffn), "w_in": f(w_in),
        "conv_wT": f(np.transpose(np.asarray(conv_w), (0, 2, 1))), "conv_b": f(conv_b),
        "conv_ln_g": f(conv_ln_g), "conv_ln_b": f(conv_ln_b), "w_out": f(w_out),
        "ffn_w_gate": f(ffn_w_gate), "ffn_w_up": f(ffn_w_up), "ffn_w_down": f(ffn_w_down),
        "moe_w_router": f(moe_w_router), "moe_w_gate": f(moe_w_gate), "moe_w_up": f(moe_w_up),
        "moe_w_down": f(moe_w_down), "maskin": mask,
    }
    import os
    cores = [int(c_) for c_ in os.environ.get("KDBG_CORES", "0,1,2,3,4,5,6,7").split(",")]
    in_maps = []
    for core in cores:
        b, c = core // 4, core % 4
        xin = np.zeros((TALL, D), np.float32)
        for j, cc in enumerate((c - 2, c - 1, c)):
            if cc >= 0:
                xin[j * 2048:(j + 1) * 2048] = x_prompt[b, cc * 2048:(cc + 1) * 2048]
        xin[NPT * 128:NPT * 128 + 64] = x_sample[core * 16:(core + 1) * 16].reshape(64, D)
        cexp = np.zeros((2, 128, D), np.float32)
        cexp[0, :, :] = c_prompt[b][None, :]
        cexp[1, 0:64, :] = np.repeat(c_sample[core * 16:(core + 1) * 16], 4, axis=0)
        flags = np.zeros((128, 4), np.float32)
        flags[:, 0] = 0.0 if c >= 2 else NEG
        flags[:, 1] = 0.0 if c >= 1 else NEG
        flags[:, 2] = 1.0 if c >= 2 else 0.0
        flags[:, 3] = 1.0 if c >= 1 else 0.0
        pos = np.zeros((TALL,), np.int64)
        pos[0:NPT * 128] = np.arange(NPT * 128) + 2048 * (c - 2)
        pos[NPT * 128:] = 2048 + (np.arange(128) % 4)
        m = dict(shared)
        m.update({
            "xin": xin, "cexp": cexp,
            "ck": np.ascontiguousarray(cache_k[:, core * 16:(core + 1) * 16].reshape(2, 16, 2048, 512)),
            "cv": np.ascontiguousarray(cache_v[:, core * 16:(core + 1) * 16].reshape(2, 16, 2048, 512)),
            "sconv": np.ascontiguousarray(state_conv[:, core * 16:(core + 1) * 16]),
            "flags": flags, "rope": _rope_tables(pos),
        })
        in_maps.append(m)
    if os.environ.get("KDBG_TRACE"):
        res = run_bass_kernel_spmd(nc, in_maps, core_ids=list(range(len(cores))), trace=True)
        print("EXEC_TIME_NS", res.exec_time_ns)
    else:
        res = run_bass_kernel_spmd(nc, in_maps, core_ids=list(range(len(cores))))
    R = dict(zip(cores, res.results))
    y_prompt = np.zeros((2, 8192, D), np.float32)
    y_sample = np.zeros((128, 4, D), np.float32)
    nkp = np.zeros((2, 2, 2048, 8, 64), np.float32); nvp = np.zeros_like(nkp)
    ncp = np.zeros((2, 2, 30, 512), np.float32)
    nks = np.zeros((2, 128, 4, 8, 64), np.float32); nvs = np.zeros_like(nks)
    ncs = np.zeros((2, 128, 30, 512), np.float32)
    for core in cores:
        b, c = core // 4, core % 4
        r = R[core]
        y_prompt[b, c * 2048:(c + 1) * 2048] = r["o_y"][0:2048]
        y_sample[core * 16:(core + 1) * 16] = r["o_y"][2048:2048 + 64].reshape(16, 4, D)
        nks[:, core * 16:(core + 1) * 16] = r["o_k"][:, 2048:2048 + 64].reshape(2, 16, 4, 8, 64)
        nvs[:, core * 16:(core + 1) * 16] = r["o_v"][:, 2048:2048 + 64].reshape(2, 16, 4, 8, 64)
        ncs[:, core * 16:(core + 1) * 16] = r["o_cs"]
        if c == 3:
            nkp[:, b] = r["o_k"][:, 0:2048].reshape(2, 2048, 8, 64)
            nvp[:, b] = r["o_v"][:, 0:2048].reshape(2, 2048, 8, 64)
            ncp[:, b] = r["o_cp"][:, 2:32]
    return (y_prompt, y_sample, nkp, nvp, ncp, nks, nvs, ncs)
```
